# Optimizing a Trainium2 kernel written in Bass

```python
import jax, jax.numpy as jnp
from jax import lax
import numpy as np

D_MODEL = 1024
BATCH = 2
SEQ = 8192
DEPTH = 1

POOL_WINDOWS = (2, 4, 8, 16)
POOL_GROUPS = len(POOL_WINDOWS)
POOL_WIDTH = D_MODEL // 2
POOL_GROUP_DIM = POOL_WIDTH // POOL_GROUPS
ATTN_HEADS = 8
HEAD_DIM = 64
ATTN_WIDTH = ATTN_HEADS * HEAD_DIM
MOBA_BLOCK = 256
MOBA_TOPK = 3
MOBA_Q_CHUNK = 64
ROPE_THETA = 500000.0
ROT_DIM = HEAD_DIM // 4
N_BRANCHES = 2
IN_PROJ_WIDTH = POOL_WIDTH + 3 * ATTN_WIDTH + N_BRANCHES * D_MODEL
PEER_HEADS = 8
PEER_NKEYS = 128
PEER_EXPERTS = PEER_NKEYS * PEER_NKEYS
PEER_QDIM = 256
PEER_HALF = PEER_QDIM // 2
PEER_TOPK = 16
PEER_TOKEN_CHUNK = 128
EPS = 1e-6
NEG = -1e30

kernel_name = "hybrid_pool_moba_peer_adaln"


def rmsnorm(x, g):
    xf = x.astype(jnp.float32)
    y = xf * lax.rsqrt(jnp.mean(xf * xf, axis=-1, keepdims=True) + EPS)
    return (y * g.astype(jnp.float32)).astype(x.dtype)


def modulate(x, g, shift, scale):
    return rmsnorm(x, g) * (1 + scale[:, None, :]) + shift[:, None, :]


def rope_partial(x, pos):
    half = ROT_DIM // 2
    inv = ROPE_THETA ** (-jnp.arange(half, dtype=jnp.float32) / half)
    ang = pos.astype(jnp.float32)[:, None] * inv[None, :]
    cos = jnp.cos(ang).astype(x.dtype)
    sin = jnp.sin(ang).astype(x.dtype)
    x1, x2, rest = x[..., :half], x[..., half:ROT_DIM], x[..., ROT_DIM:]
    return jnp.concatenate([x1 * cos - x2 * sin, x1 * sin + x2 * cos, rest], axis=-1)


def pool_mixer(u, pool_w, pool_scale):
    B, S, _ = u.shape
    uf = u.astype(jnp.float32)
    cs = jnp.cumsum(uf, axis=1)
    cs = jnp.concatenate([jnp.zeros_like(cs[:, :1]), cs], axis=1)
    pos = jnp.arange(S)
    groups = []
    for g, w in enumerate(POOL_WINDOWS):
        sl = slice(g * POOL_GROUP_DIM, (g + 1) * POOL_GROUP_DIM)
        lo = jnp.maximum(pos + 1 - w, 0)
        win_sum = cs[:, 1:, sl] - cs[:, lo, sl]
        cnt = jnp.minimum(pos + 1, w).astype(jnp.float32)[None, :, None]
        groups.append(win_sum / cnt)
    pooled = jnp.stack(groups, axis=2)
    diff = pooled - uf.reshape(B, S, POOL_GROUPS, POOL_GROUP_DIM)
    mixed = jnp.einsum('bsgi,gio->bsgo', diff, pool_w.astype(jnp.float32))
    mixed = mixed.reshape(B, S, POOL_WIDTH) * pool_scale.astype(jnp.float32)
    return mixed.astype(u.dtype)


def moba_attention(q, k, v):
    B, H, S, dh = q.shape
    n_blocks = -(-S // MOBA_BLOCK)
    pad = n_blocks * MOBA_BLOCK - S
    kp = jnp.pad(k, ((0, 0), (0, 0), (0, pad), (0, 0)))
    vp = jnp.pad(v, ((0, 0), (0, 0), (0, pad), (0, 0)))
    k_blocks = kp.reshape(B, H, n_blocks, MOBA_BLOCK, dh)
    v_blocks = vp.reshape(B, H, n_blocks, MOBA_BLOCK, dh)
    k_mean = jnp.mean(k_blocks.astype(jnp.float32), axis=3)
    topk = min(MOBA_TOPK, n_blocks)
    scale = HEAD_DIM ** -0.5
    bi = jnp.arange(B)[:, None, None, None]
    hi = jnp.arange(H)[None, :, None, None]
    blk_ids = jnp.arange(n_blocks)
    key_off = jnp.arange(MOBA_BLOCK)

    def chunk(start):
        q_c = lax.dynamic_slice_in_dim(q, start, MOBA_Q_CHUNK, axis=2)
        q_pos = start + jnp.arange(MOBA_Q_CHUNK)
        q_blk = q_pos // MOBA_BLOCK
        gate = jnp.einsum('bhcd,bhnd->bhcn', q_c.astype(jnp.float32), k_mean)
        past = blk_ids[None, :] < q_blk[:, None]
        gate = jnp.where(past, gate, -jnp.inf)
        _, sel = lax.top_k(gate, topk)
        sel_valid = sel < q_blk[:, None]
        k_sel = k_blocks[bi, hi, sel]
        v_sel = v_blocks[bi, hi, sel]
        s_past = jnp.einsum('bhcd,bhcjld->bhcjl', q_c, k_sel).astype(jnp.float32) * scale
        s_past = jnp.where(sel_valid[..., None], s_past, NEG)
        own = start // MOBA_BLOCK
        k_own = lax.dynamic_index_in_dim(k_blocks, own, axis=2, keepdims=False)
        v_own = lax.dynamic_index_in_dim(v_blocks, own, axis=2, keepdims=False)
        s_own = jnp.einsum('bhcd,bhld->bhcl', q_c, k_own).astype(jnp.float32) * scale
        k_pos = own * MOBA_BLOCK + key_off
        s_own = jnp.where(k_pos[None, :] <= q_pos[:, None], s_own, NEG)
        s = jnp.concatenate([s_past.reshape(B, H, MOBA_Q_CHUNK, topk * MOBA_BLOCK), s_own], axis=-1)
        p = jax.nn.softmax(s, axis=-1)
        p_past = p[..., :topk * MOBA_BLOCK].reshape(B, H, MOBA_Q_CHUNK, topk, MOBA_BLOCK).astype(v.dtype)
        p_own = p[..., topk * MOBA_BLOCK:].astype(v.dtype)
        return (jnp.einsum('bhcjl,bhcjld->bhcd', p_past, v_sel)
                + jnp.einsum('bhcl,bhld->bhcd', p_own, v_own))

    starts = jnp.arange(S // MOBA_Q_CHUNK) * MOBA_Q_CHUNK
    out = lax.map(chunk, starts)
    return out.transpose(1, 2, 0, 3, 4).reshape(B, H, S, dh)


def peer_ffn(h, w_q, sub_keys, u_tab, v_tab):
    B, S, D = h.shape
    T = B * S
    hf = h.reshape(T, D)
    q = (hf @ w_q).reshape(T, PEER_HEADS, 2, PEER_HALF)
    s = jnp.einsum('thpd,hpnd->thpn', q, sub_keys).astype(jnp.float32)
    sv, si = lax.top_k(s, PEER_TOPK)
    cand = sv[:, :, 0, :, None] + sv[:, :, 1, None, :]
    cand_idx = si[:, :, 0, :, None] * PEER_NKEYS + si[:, :, 1, None, :]
    cv, ci = lax.top_k(cand.reshape(T, PEER_HEADS, PEER_TOPK * PEER_TOPK), PEER_TOPK)
    expert = jnp.take_along_axis(cand_idx.reshape(T, PEER_HEADS, PEER_TOPK * PEER_TOPK), ci, axis=-1)
    g = jax.nn.softmax(cv, axis=-1)
    nc = T // PEER_TOKEN_CHUNK

    def chunk(args):
        x_c, e_c, g_c = args
        u = u_tab[e_c]
        vv = v_tab[e_c]
        a = jax.nn.gelu(jnp.einsum('cd,chkd->chk', x_c, u).astype(jnp.float32), approximate=False)
        return jnp.einsum('chk,chkd->cd', (g_c * a).astype(vv.dtype), vv)

    out = lax.map(chunk, (hf.reshape(nc, PEER_TOKEN_CHUNK, D),
                          expert.reshape(nc, PEER_TOKEN_CHUNK, PEER_HEADS, PEER_TOPK),
                          g.reshape(nc, PEER_TOKEN_CHUNK, PEER_HEADS, PEER_TOPK)))
    return out.reshape(B, S, D).astype(h.dtype)


def setup_inputs(seed: int = 0) -> dict:
    key = jax.random.key(seed)
    ks = jax.random.split(key, 20)
    D = D_MODEL
    nrm = lambda k, shape, s: jax.random.normal(k, shape, jnp.float32) * s
    return {
        "x": nrm(ks[0], (BATCH, SEQ, D), 1.0),
        "c": nrm(ks[1], (BATCH, D), 1.0),
        "w_ada": nrm(ks[2], (DEPTH, D, 6 * D), 0.5 * D ** -0.5),
        "b_ada": nrm(ks[3], (DEPTH, 6 * D), 0.01),
        "norm_mix_g": 1.0 + nrm(ks[4], (DEPTH, D), 0.02),
        "w_in": nrm(ks[5], (DEPTH, D, IN_PROJ_WIDTH), D ** -0.5),
        "pool_w": nrm(ks[6], (DEPTH, POOL_GROUPS, POOL_GROUP_DIM, POOL_GROUP_DIM), POOL_GROUP_DIM ** -0.5),
        "pool_scale": 1.0 + nrm(ks[7], (DEPTH, POOL_WIDTH), 0.1),
        "w_branch_pool": nrm(ks[8], (DEPTH, POOL_WIDTH, D), POOL_WIDTH ** -0.5),
        "w_branch_attn": nrm(ks[9], (DEPTH, ATTN_WIDTH, D), ATTN_WIDTH ** -0.5),
        "w_out": nrm(ks[10], (DEPTH, D, D), D ** -0.5),
        "norm_ffn_g": 1.0 + nrm(ks[11], (DEPTH, D), 0.02),
        "peer_wq": nrm(ks[12], (DEPTH, D, PEER_HEADS * PEER_QDIM), D ** -0.5),
        "peer_sub_keys": nrm(ks[13], (DEPTH, PEER_HEADS, 2, PEER_NKEYS, PEER_HALF), PEER_HALF ** -0.5),
        "peer_u": nrm(ks[14], (DEPTH, PEER_EXPERTS, D), D ** -0.5),
        "peer_v": nrm(ks[15], (DEPTH, PEER_EXPERTS, D), 0.5),
        "norm_final_g": 1.0 + nrm(ks[16], (D,), 0.02),
    }


def reference(x, c, w_ada, b_ada, norm_mix_g, w_in, pool_w, pool_scale, w_branch_pool,
              w_branch_attn, w_out, norm_ffn_g, peer_wq, peer_sub_keys, peer_u, peer_v,
              norm_final_g):
    B, S, D = x.shape
    pos = jnp.arange(S)
    c_act = jax.nn.silu(c)
    for l in range(DEPTH):
        mod = c_act @ w_ada[l] + b_ada[l]
        shift1, scale1, gate1, shift2, scale2, gate2 = jnp.split(mod, 6, axis=-1)

        h = modulate(x, norm_mix_g[l], shift1, scale1)
        proj = h @ w_in[l]
        u_pool = proj[..., :POOL_WIDTH]
        qkv = proj[..., POOL_WIDTH:POOL_WIDTH + 3 * ATTN_WIDTH]
        gates = proj[..., POOL_WIDTH + 3 * ATTN_WIDTH:]
        qkv = qkv.reshape(B, S, 3, ATTN_HEADS, HEAD_DIM).transpose(2, 0, 3, 1, 4)
        q = rope_partial(qkv[0], pos)
        k = rope_partial(qkv[1], pos)
        attn = moba_attention(q, k, qkv[2]).transpose(0, 2, 1, 3).reshape(B, S, ATTN_WIDTH)
        pooled = pool_mixer(u_pool, pool_w[l], pool_scale[l])
        g_pool = jax.nn.sigmoid(gates[..., :D])
        g_attn = jax.nn.sigmoid(gates[..., D:])
        merged = g_pool * (pooled @ w_branch_pool[l]) + g_attn * (attn @ w_branch_attn[l])
        x = x + gate1[:, None, :] * (merged @ w_out[l])

        h2 = modulate(x, norm_ffn_g[l], shift2, scale2)
        x = x + gate2[:, None, :] * peer_ffn(h2, peer_wq[l], peer_sub_keys[l], peer_u[l], peer_v[l])
    return rmsnorm(x, norm_final_g)
```

```python
import numpy as np
from contextlib import ExitStack
import concourse.bass as bass
import concourse.mybir as mybir
from concourse.bass_utils import run_bass_kernel_spmd

F32 = mybir.dt.float32
BF16 = mybir.dt.bfloat16
U32 = mybir.dt.uint32
ALU = mybir.AluOpType
AF = mybir.ActivationFunctionType
AX = mybir.AxisListType

D = 1024
S = 8192
TOK = 2048
NCH = 16
EPS = 1e-6
NEGM = -30000.0


class Buf:
    __slots__ = ("w", "r")

    def __init__(self):
        self.w = None
        self.r = {}


class Prog:
    ENG = ("pe", "act", "dve", "pool", "sp")

    def __init__(self, nc, stack, lanes_sp=12, lanes_pool=8, lanes_act=2):
        self.nc = nc
        self.eng = {"pe": nc.tensor, "act": nc.scalar, "dve": nc.vector, "pool": nc.gpsimd, "sp": nc.sync}
        self.sem = {}
        self.cnt = {}
        for e in self.ENG:
            self.sem[e] = stack.enter_context(nc.semaphore("s_" + e))
            self.cnt[e] = 0
        self.lanes = {"sp": [], "pool": [], "act": []}
        for q, n in (("sp", lanes_sp), ("pool", lanes_pool), ("act", lanes_act)):
            for l in range(n):
                s = "L%s%d" % (q, l)
                self.sem[s] = stack.enter_context(nc.semaphore("s_" + s))
                self.cnt[s] = 0
                self.lanes[q].append(s)
        self.rr = {"sp": 0, "pool": 0, "act": 0}
        self.waited = {e: {} for e in self.ENG}

    def _deps(self, reads, writes):
        deps = {}

        def add(s, v):
            if deps.get(s, 0) < v:
                deps[s] = v

        for b in reads:
            if b.w is not None:
                add(*b.w)
        for b in writes:
            if b.w is not None:
                add(*b.w)
            for s, v in b.r.items():
                add(s, v)
        return deps

    def _wait(self, e, deps):
        E = self.eng[e]
        for s, v in deps.items():
            if e == "pe" and s == "pe":
                continue
            if self.waited[e].get(s, 0) >= v:
                continue
            if s == e:
                v = max(v, self.cnt[e] - 4)
            E.wait_ge(self.sem[s], v)
            self.waited[e][s] = v

    def op(self, e, fn, reads=(), writes=()):
        self._wait(e, self._deps(reads, writes))
        ins = fn(self.eng[e])
        self.cnt[e] += 1
        ins.then_inc(self.sem[e], 1)
        c = self.cnt[e]
        for b in reads:
            b.r[e] = c
        for b in writes:
            b.w = (e, c)
            b.r = {}

    def dma(self, q, out, in_, reads=(), writes=()):
        ls = self.lanes[q]
        s = ls[self.rr[q]]
        self.rr[q] = (self.rr[q] + 1) % len(ls)
        deps = self._deps(reads, writes)
        if self.cnt[s] > 0 and deps.get(s, 0) < self.cnt[s]:
            deps[s] = self.cnt[s]
        self._wait(q, deps)
        ins = self.eng[q].dma_start(out=out, in_=in_)
        self.cnt[s] += 16
        ins.then_inc(self.sem[s], 16)
        c = self.cnt[s]
        for b in reads:
            b.r[s] = c
        for b in writes:
            b.w = (s, c)
            b.r = {}

    def barrier(self, nopool=False):
        for e in self.ENG:
            if nopool and e == "pool":
                continue
            deps = {s: v for s, v in self.cnt.items() if v > 0 and s != e
                    and not (nopool and (s == "pool" or s.startswith("Lpool")))}
            self._wait(e, deps)


def build(stop_after=None, debug=False):
    nc = bass.Bass("TRN2", target_bir_lowering=False)
    di = lambda name, shape, dt=F32: nc.dram_tensor(name, list(shape), dt, kind="ExternalInput").ap()
    xT = di("xT", [D, S]); xhT = di("xhT", [D, 16])
    cosF = di("cosF", [128, S]); sinF = di("sinF", [128, S])
    cT = di("cT", [128, 8]); badaT = di("badaT", [128, 48])
    gmixT = di("gmixT", [128, 8]); gffnT = di("gffnT", [128, 8]); gfinT = di("gfinT", [128, 8])
    pscT = di("pscT", [128, 4])
    w_ada = di("w_ada", [D, 6 * D]); w_in = di("w_in", [D, 4096]); w_sw = di("w_sw", [D, 1024])
    pool_w = di("pool_w", [4, 128, 128]); wbp = di("wbp", [512, D]); wba = di("wba", [512, D])
    w_out = di("w_out", [D, D]); wq = di("wq", [D, 2048]); skT = di("skT", [128, 16, 128])
    uT = di("uT", [D, 16384]); vtab = di("vtab", [16384, D])
    pastb = di("pastb", [1, 24]); invcnt = di("invcnt", [1, 64]); haloflag = di("haloflag", [1, 1])
    outT = nc.dram_tensor("outT", [D, TOK], F32, kind="ExternalOutput").ap()
    dscr = lambda name, shape, dt: nc.dram_tensor(name, list(shape), dt, kind="Internal").ap()
    kT_scr = dscr("kT_scr", [8, 64, S], BF16)
    v_scr = dscr("v_scr", [S, 512], BF16)
    x1_scr = dscr("x1_scr", [D, TOK], F32)
    uT_scr = dscr("uT_scr", [32, 128, 8, 512], BF16)
    v2_scr = dscr("v2_scr", [32, 128, 4, 1024], BF16)
    dbg = {}

    def dbg_out(name, shape, dt=F32):
        dbg[name] = nc.dram_tensor("dbg_" + name, list(shape), dt, kind="ExternalOutput").ap()
        return dbg[name]

    with ExitStack() as top:
        P = Prog(nc, top)
        T0 = lambda name, shape, dt: top.enter_context(nc.sbuf_tensor(name, list(shape), dt))
        pairs = [top.enter_context(nc.psum_tensor("pair%d" % i, [128, 1024], F32)) for i in range(4)]
        banks = [pairs[i // 2][:, (i % 2) * 512:(i % 2 + 1) * 512] for i in range(8)]
        bbuf = [Buf() for _ in range(8)]
        rot = {"i": 0, "set": list(range(8))}
        rcnt = {}

        def pb():
            s = rot["set"]
            rot["i"] = (rot["i"] + 1) % len(s)
            k = s[rot["i"]]
            return banks[k], bbuf[k]

        def rotn(name, n=2):
            rcnt[name] = (rcnt.get(name, -1) + 1) % n
            return rcnt[name]

        ones_f = T0("ones_f", [128, 128], F32)
        ident_f = T0("ident_f", [128, 128], F32)
        ident_b = T0("ident_b", [128, 128], BF16)
        iota_f = T0("iota_f", [128, 128], F32)
        iota_b = T0("iota_b", [128, 128], BF16)
        pidx64 = T0("pidx64", [128, 1], F32)
        tri = T0("tri", [128, 2, 256], BF16)
        modT = T0("modT", [128, 48], F32)
        gs1 = T0("gs1", [128, 8], F32); gs2 = T0("gs2", [128, 8], F32)
        gfin = T0("gfin", [128, 8], F32)
        psc = T0("psc", [128, 4], F32)
        hflag = T0("hflag", [128, 1], F32)
        epsT = T0("epsT", [128, 1], F32)
        sqr = [T0("sqr%d" % i, [128, 512], F32) for i in range(2)]
        B_sqr = [Buf(), Buf()]
        sqb = [T0("sqb%d" % i, [128, 512], BF16) for i in range(2)]
        B_sqb = [Buf(), Buf()]
        ones_b = T0("ones_b", [128, 128], BF16)
        B_const = Buf(); B_mod = Buf()

        P.op("dve", lambda e: e.memset(ones_f[:], 1.0), writes=[B_const])
        P.op("dve", lambda e: e.memset(ones_b[:], 1.0), writes=[B_const])
        P.op("dve", lambda e: e.memset(epsT[:], EPS), writes=[B_const])
        P.op("pool", lambda e: e.memset(ident_f[:], 0.0), writes=[B_const])
        P.op("pool", lambda e: e.affine_select(ident_f[:], ident_f[:], pattern=[[-1, 128]], compare_op=ALU.not_equal,
                                               fill=1.0, base=0, channel_multiplier=1), reads=[B_const], writes=[B_const])
        P.op("pool", lambda e: e.iota(iota_f[:], pattern=[[1, 128]], base=0, channel_multiplier=0,
                                      allow_small_or_imprecise_dtypes=True), writes=[B_const])
        P.op("pool", lambda e: e.iota(pidx64[:], pattern=[[0, 1]], base=-64, channel_multiplier=1,
                                      allow_small_or_imprecise_dtypes=True), writes=[B_const])
        P.op("pool", lambda e: e.memset(tri[:], 1.0), writes=[B_const])
        for half in range(2):
            P.op("pool", lambda e, half=half: e.affine_select(tri[:, half, :], tri[:, half, :], pattern=[[1, 256]], compare_op=ALU.is_ge,
                                                              fill=0.0, base=-128 * half, channel_multiplier=-1), reads=[B_const], writes=[B_const])
        P.op("dve", lambda e: e.tensor_copy(ident_b[:], ident_f[:]), reads=[B_const], writes=[B_const])
        P.op("dve", lambda e: e.tensor_copy(iota_b[:], iota_f[:]), reads=[B_const], writes=[B_const])
        thr16 = T0("thr16", [128, 16], F32)
        P.op("dve", lambda e: e.tensor_scalar(thr16[:], iota_f[:, 0:16], 16.0, 16.0, op0=ALU.mult, op1=ALU.add), reads=[B_const], writes=[B_const])
        P.dma("sp", gfin[:], gfinT[:, :], writes=[B_const])
        P.dma("sp", psc[:], pscT[:, :], writes=[B_const])
        P.dma("sp", hflag[:], haloflag.partition_broadcast(128), writes=[B_const])

        def rms_rstd(src, n, rstd, B_src, B_rstd):
            bk, bb = pb()
            for dc in range(8):
                i = rotn("sqb")
                P.op("act", lambda e, dc=dc, i=i: e.activation(sqb[i][:, 0:n], src(dc), AF.Square), reads=[B_src], writes=[B_sqb[i]])
                P.op("pe", lambda e, dc=dc, i=i: e.matmul(bk[:, 0:n], lhsT=ones_b[:], rhs=sqb[i][:, 0:n], start=(dc == 0), stop=(dc == 7)),
                     reads=[B_sqb[i], B_const], writes=[bb])
            P.op("act", lambda e: e.activation(rstd, bk[:, 0:n], AF.Sqrt, bias=epsT[:], scale=1.0 / D), reads=[bb, B_const], writes=[B_rstd])
            P.op("dve", lambda e: e.reciprocal(rstd, rstd), reads=[B_rstd], writes=[B_rstd])

        def modulate(src, n, rstd, dst, B_src, B_rstd, B_dst, shift, gs):
            for dc in range(8):
                i = rotn("sqr")
                P.op("dve", lambda e, dc=dc, i=i: e.tensor_tensor(sqr[i][:, 0:n], src(dc), rstd, ALU.mult), reads=[B_src, B_rstd], writes=[B_sqr[i]])
                P.op("act", lambda e, dc=dc, i=i: e.activation(dst(dc), sqr[i][:, 0:n], AF.Identity, bias=shift(dc), scale=gs[:, dc:dc + 1]),
                     reads=[B_sqr[i], B_mod], writes=[B_dst])

        with ExitStack() as ph:
            T = lambda name, shape, dt: ph.enter_context(nc.sbuf_tensor(name, list(shape), dt))
            cact = T("cact", [128, 8], F32); bada = T("bada", [128, 48], F32)
            gmix = T("gmix", [128, 8], F32); gffn = T("gffn", [128, 8], F32)
            tmp8 = T("tmp8", [128, 8], F32)
            wa = [T("wa%d" % i, [128, 8, 1024], F32) for i in range(3)]
            B_wa = [Buf(), Buf(), Buf()]; B_c = Buf()
            P.dma("sp", cact[:], cT[:, :], writes=[B_c])
            P.dma("sp", bada[:], badaT[:, :], writes=[B_c])
            P.dma("sp", gmix[:], gmixT[:, :], writes=[B_c])
            P.dma("sp", gffn[:], gffnT[:, :], writes=[B_c])
            P.op("act", lambda e: e.activation(cact[:], cact[:], AF.Silu), reads=[B_c], writes=[B_c])
            w_ada_v = w_ada.rearrange("(dc p) f -> p dc f", p=128)
            mbk, mbb = pb()
            for pc in range(6):
                w_, bw = wa[pc % 3], B_wa[pc % 3]
                P.dma("sp", w_[:], w_ada_v[:, :, pc * 1024:(pc + 1) * 1024], writes=[bw])
                for fl in range(8):
                    fc = pc * 8 + fl
                    for dc in range(8):
                        P.op("pe", lambda e, w_=w_, fl=fl, fc=fc, dc=dc: e.matmul(
                            mbk[:, fc:fc + 1], lhsT=w_[:, dc, fl * 128:(fl + 1) * 128], rhs=cact[:, dc:dc + 1],
                            start=(dc == 0), stop=(dc == 7)), reads=[bw, B_c], writes=[mbb])
            P.op("dve", lambda e: e.tensor_tensor(modT[:], mbk[:, 0:48], bada[:], ALU.add), reads=[mbb, B_c], writes=[B_mod])
            P.op("dve", lambda e: e.tensor_scalar(tmp8[:], modT[:, 8:16], 1.0, None, op0=ALU.add), reads=[B_mod], writes=[B_c])
            P.op("dve", lambda e: e.tensor_tensor(gs1[:], tmp8[:], gmix[:], ALU.mult), reads=[B_c], writes=[B_mod])
            P.op("dve", lambda e: e.tensor_scalar(tmp8[:], modT[:, 32:40], 1.0, None, op0=ALU.add), reads=[B_mod, B_c], writes=[B_c])
            P.op("dve", lambda e: e.tensor_tensor(gs2[:], tmp8[:], gffn[:], ALU.mult), reads=[B_c], writes=[B_mod])
            if debug:
                o = dbg_out("modT", [128, 48])
                P.dma("sp", o[:, :], modT[:], reads=[B_mod])
            P.barrier(nopool=True)
        SHIFT1 = lambda dc: modT[:, dc:dc + 1]
        GATE1 = lambda dc: modT[:, 16 + dc:17 + dc]
        SHIFT2 = lambda dc: modT[:, 24 + dc:25 + dc]
        GATE2 = lambda dc: modT[:, 40 + dc:41 + dc]
        if stop_after == 0:
            return nc, dbg

        conv = ExitStack()
        NCV = 4
        CW = 512
        cvf = [conv.enter_context(nc.sbuf_tensor("cvf%d" % i, [128, CW], F32, side="right")) for i in range(NCV)]
        cvb = [conv.enter_context(nc.sbuf_tensor("cvb%d" % i, [128, CW], BF16, side="right")) for i in range(NCV)]
        B_cvf = [Buf() for _ in range(NCV)]; B_cvb = [Buf() for _ in range(NCV)]
        B_uTs = Buf(); B_v2s = Buf()
        def emit_conversion():
            if stop_after is None or stop_after >= 4:
                P._wait("pool", {s_: v_ for s_, v_ in P.cnt.items() if v_ > 0 and s_ in ("pe", "act", "dve")})
                k = 0
                for (src, dst, bd, nrow) in ((uT, uT_scr, B_uTs, 8), (vtab, v2_scr, B_v2s, 128)):
                    srcv = src.rearrange("(r p) c -> r p c", p=128)
                    is_u = (nrow == 8)
                    ncol = src.shape[1] // CW
                    jobs_cv = [(r, cc) for r in range(nrow) for cc in range(ncol)]
                    for n_, (r, cc) in enumerate(jobs_cv[:NCV - 1]):
                        P.dma("pool", cvf[(k + n_) % NCV][:], srcv[r, :, cc * CW:(cc + 1) * CW], writes=[B_cvf[(k + n_) % NCV]])
                    for n_, (r, cc) in enumerate(jobs_cv):
                        if n_ + NCV - 1 < len(jobs_cv):
                            r2, cc2 = jobs_cv[n_ + NCV - 1]
                            P.dma("pool", cvf[(k + NCV - 1) % NCV][:], srcv[r2, :, cc2 * CW:(cc2 + 1) * CW], writes=[B_cvf[(k + NCV - 1) % NCV]])
                        f, b_ = cvf[k % NCV], cvb[k % NCV]
                        P.op("pool", lambda e, f=f, b_=b_: e.tensor_copy(b_[:], f[:]), reads=[B_cvf[k % NCV]], writes=[B_cvb[k % NCV]])
                        dst_ap = dst[cc, :, r, :] if is_u else dst[r // 4, :, r % 4, cc * CW:(cc + 1) * CW]
                        P.dma("pool", dst_ap, b_[:], reads=[B_cvb[k % NCV]], writes=[bd])
                        k += 1

        mid = ExitStack()
        attnT = mid.enter_context(nc.sbuf_tensor("attnT", [128, 4, TOK], BF16))
        B_att = Buf()
        midq = ExitStack()
        TM = lambda name, shape, dt: midq.enter_context(nc.sbuf_tensor(name, list(shape), dt))
        Qaug = TM("Qaug", [96, 8, TOK], BF16)
        kmT = TM("kmT", [64, 8, 32], BF16)
        B_Q = [Buf() for _ in range(8)]; B_Qm = [Buf() for _ in range(8)]; B_km = Buf()
        B_kscr = Buf(); B_vscr = Buf()
        xT_v = xT.rearrange("(dc p) t -> p dc t", p=128)

        with ExitStack() as ph:
            T = lambda name, shape, dt: ph.enter_context(nc.sbuf_tensor(name, list(shape), dt))
            win = T("win", [128, 8, 1536], BF16)
            wsw = T("wsw", [128, 8, 1024], BF16)
            xc = [T("xc%d" % i, [128, 8, 512], F32) for i in range(2)]; B_xc = [Buf(), Buf()]
            B_w = Buf()
            kms = T("kms", [128, 4, 32], F32); B_kms = Buf()
            pieces = []
            w_in_v = w_in.rearrange("(dc p) f -> p dc f", p=128)
            for i in range(3):
                pieces.append((w_in_v[:, :, 512 + i * 512:512 + (i + 1) * 512], win[:, :, i * 512:(i + 1) * 512]))
            w_sw_v = w_sw.rearrange("(dc p) f -> p dc f", p=128)
            for i in range(2):
                pieces.append((w_sw_v[:, :, i * 512:(i + 1) * 512], wsw[:, :, i * 512:(i + 1) * 512]))
            for i, (src, dst) in enumerate(pieces):
                st_, bs = xc[i % 2], B_xc[i % 2]
                P.dma("sp", st_[:], src, writes=[bs])
                if i % 2 == 0:
                    P.op("dve", lambda e, dst=dst, st_=st_: e.tensor_copy(dst, st_[:]), reads=[bs], writes=[B_w])
                else:
                    P.op("act", lambda e, dst=dst, st_=st_: e.copy(dst, st_[:]), reads=[bs], writes=[B_w])
            cs = [T("cs%d" % i, [128, 2, 512], F32) for i in range(2)]; B_cs = [Buf(), Buf()]
            rstd = T("rstd", [128, 512], F32); B_rstd = Buf()
            hT = [T("hT%d" % i, [128, 8, 512], BF16) for i in range(2)]; B_hT = [Buf(), Buf()]
            t1 = [T("t1_%d" % i, [128, 512], F32) for i in range(2)]; B_t1 = [Buf(), Buf()]
            t2 = [T("t2_%d" % i, [128, 512], F32) for i in range(2)]; B_t2 = [Buf(), Buf()]
            kb = [T("kb%d" % i, [128, 512], BF16) for i in range(4)]; B_kb = [Buf() for _ in range(4)]
            vb = [T("vb%d" % i, [128, 512], BF16) for i in range(4)]; B_vb = [Buf() for _ in range(4)]

            def load_x(ci):
                P.dma("sp", xc[ci % 2][:], xT_v[:, :, ci * 512:(ci + 1) * 512], writes=[B_xc[ci % 2]])

            def load_cs(ci):
                P.dma("sp", cs[ci % 2][:, 0, :], cosF[:, ci * 512:(ci + 1) * 512], writes=[B_cs[ci % 2]])
                P.dma("sp", cs[ci % 2][:, 1, :], sinF[:, ci * 512:(ci + 1) * 512], writes=[B_cs[ci % 2]])

            def norm(ci):
                x_, bx = xc[ci % 2], B_xc[ci % 2]
                h_, bh = hT[ci % 2], B_hT[ci % 2]
                rms_rstd(lambda dc: x_[:, dc, :], 512, rstd[:], bx, B_rstd)
                modulate(lambda dc: x_[:, dc, :], 512, rstd[:], lambda dc: h_[:, dc, :], bx, B_rstd, bh, SHIFT1, gs1)

            def proj(ci):
                own = ci >= 12
                oc0 = (ci - 12) * 512
                c_, bc_ = cs[ci % 2], B_cs[ci % 2]
                h_, bh = hT[ci % 2], B_hT[ci % 2]

                def proj_fm(col0, wt, bk):
                    for dc in range(8):
                        P.op("pe", lambda e, dc=dc: e.matmul(bk[0][:, :], lhsT=wt[:, dc, col0:col0 + 128], rhs=h_[:, dc, :],
                                                             start=(dc == 0), stop=(dc == 7)), reads=[B_w, bh], writes=[bk[1]])

                def rope_pair(col_main, col_sw):
                    k1 = pb(); proj_fm(col_main, win, k1)
                    k2 = pb(); proj_fm(col_sw, wsw, k2)
                    i = rotn("t1")
                    P.op("dve", lambda e: e.tensor_tensor(t1[i][:], k1[0][:, :], c_[:, 0, :], ALU.mult), reads=[k1[1], bc_], writes=[B_t1[i]])
                    P.op("dve", lambda e: e.tensor_tensor(t2[i][:], k2[0][:, :], c_[:, 1, :], ALU.mult), reads=[k2[1], bc_], writes=[B_t2[i]])
                    P.op("dve", lambda e: e.tensor_tensor(t1[i][:], t1[i][:], t2[i][:], ALU.add), reads=[B_t1[i], B_t2[i]], writes=[B_t1[i]])
                    return i

                for m in range(4):
                    i = rope_pair(512 + m * 128, 512 + m * 128)
                    P.op("dve", lambda e, m=m, i=i: e.tensor_reduce(kms[:, m, 2 * ci:2 * ci + 2], t1[i][:].rearrange("p (b k) -> p b k", b=2),
                                                                    AX.X, ALU.add), reads=[B_t1[i]], writes=[B_kms])
                    j = rotn("kb", 4)
                    P.op("act", lambda e, i=i, j=j: e.copy(kb[j][:], t1[i][:]), reads=[B_t1[i]], writes=[B_kb[j]])
                    P.dma("sp", kT_scr[2 * m, :, ci * 512:(ci + 1) * 512], kb[j][0:64, :], reads=[B_kb[j]], writes=[B_kscr])
                    P.dma("sp", kT_scr[2 * m + 1, :, ci * 512:(ci + 1) * 512], kb[j][64:128, :], reads=[B_kb[j]], writes=[B_kscr])
                for tt in range(4):
                    bk, bb = pb()
                    for dc in range(8):
                        P.op("pe", lambda e, dc=dc, tt=tt, bk=bk: e.matmul(bk[:, :], lhsT=h_[:, dc, tt * 128:(tt + 1) * 128], rhs=win[:, dc, 1024:1536],
                                                                           start=(dc == 0), stop=(dc == 7)), reads=[B_w, bh], writes=[bb])
                    j = rotn("vb", 4)
                    P.op("act", lambda e, j=j, bk=bk: e.copy(vb[j][:], bk[:, :]), reads=[bb], writes=[B_vb[j]])
                    tok0 = ci * 512 + tt * 128
                    P.dma("sp", v_scr[tok0:tok0 + 128, :], vb[j][:], reads=[B_vb[j]], writes=[B_vscr])
                if not own:
                    return
                for m in range(4):
                    i = rope_pair(m * 128, m * 128)
                    P.op("act", lambda e, i=i, m=m: e.mul(Qaug[0:64, 2 * m, oc0:oc0 + 512], t1[i][0:64, :], 0.125), reads=[B_t1[i]], writes=[B_Q[2 * m]])
                    P.op("act", lambda e, i=i, m=m: e.mul(Qaug[0:64, 2 * m + 1, oc0:oc0 + 512], t1[i][64:128, :], 0.125), reads=[B_t1[i]], writes=[B_Q[2 * m + 1]])

            load_x(0); load_x(1); load_cs(0)
            norm(0)
            for ci in range(NCH):
                if ci + 1 < NCH:
                    load_cs(ci + 1)
                    norm(ci + 1)
                if ci + 2 < NCH:
                    load_x(ci + 2)
                proj(ci)
            for m in range(4):
                P.op("dve", lambda e, m=m: e.tensor_scalar(kmT[:, 2 * m, :], kms[0:64, m, :], 1.0 / 256, None, op0=ALU.mult), reads=[B_kms], writes=[B_km])
                P.op("act", lambda e, m=m: e.mul(kmT[:, 2 * m + 1, :], kms[64:128, m, :], 1.0 / 256), reads=[B_kms], writes=[B_km])
            if debug:
                o = dbg_out("Qaug", [96, 8, TOK], BF16)
                P.dma("sp", o, Qaug[:], reads=B_Q)
                o = dbg_out("kmT", [64, 8, 32], BF16)
                P.dma("sp", o, kmT[:], reads=[B_km])
                o = dbg_out("kT", [8, 64, S], BF16)
                P.dma("sp", o, kT_scr, reads=[B_kscr])
                o = dbg_out("v", [S, 512], BF16)
                P.dma("sp", o, v_scr, reads=[B_vscr])
            P.barrier(nopool=True)
        emit_conversion()
        if stop_after == 1:
            conv.close(); midq.close(); mid.close()
            return nc, dbg

        c1w = ExitStack()
        TR = lambda name, shape, dt: c1w.enter_context(nc.sbuf_tensor(name, list(shape), dt, side="right"))
        wpg = TR("wpg", [128, 8, 2560], BF16)
        pw = TR("pw", [128, 4, 128], BF16)
        wbpb = TR("wbpb", [128, 4, 1024], BF16)
        wbab = TR("wbab", [128, 4, 1024], BF16)
        woutb = TR("woutb", [128, 8, 1024], BF16)
        B_wc1 = Buf()
        with ExitStack() as ph:
            T = lambda name, shape, dt: ph.enter_context(nc.sbuf_tensor(name, list(shape), dt))
            Kaug = [T("Kaug%d" % i, [96, S], BF16) for i in range(2)]; B_K = [Buf(), Buf()]
            Vaug = [T("Vaug%d" % i, [128, 64, 65], BF16) for i in range(2)]; B_V = [Buf(), Buf()]
            Pt = [T("Pt%d" % i, [128, 2, 512], BF16) for i in range(3)]; B_Pt = [Buf() for _ in range(3)]
            gbias = T("gbias", [128, 8, 32], F32)
            gb = [T("gb%d" % i, [128, 8, 32], F32) for i in range(2)]; B_gb = [Buf(), Buf()]
            mx8 = [T("mx8%d" % i, [128, 8, 8], F32) for i in range(2)]; B_mx = [[Buf() for _ in range(8)] for _ in range(2)]
            thr = [T("thr%d" % i, [128, 8, 1], F32) for i in range(2)]; B_thr = [Buf(), Buf()]
            stage = [T("stage%d" % i, [128, 8, 96], BF16) for i in range(2)]; B_stage = [Buf(), Buf()]
            Osb = T("Osb", [65, 512], F32); B_O = Buf()
            B_c2 = Buf()
            for i in range(2):
                P.op("dve", lambda e, i=i: e.tensor_scalar(Kaug[i][64:96, :].rearrange("p (b k) -> p b k", b=32),
                                                           iota_f[64:96, 0:32].unsqueeze(2).to_broadcast([32, 32, 256]),
                                                           pidx64[64:96, 0:1], None, op0=ALU.is_equal), reads=[B_const], writes=[B_K[i]])
            P.op("dve", lambda e: e.memset(gbias[:], -1e30), writes=[B_c2])
            for s_ in range(8):
                P.dma("sp", gbias[:, s_, 0:24], pastb.partition_broadcast(128), reads=[B_c2], writes=[B_c2])
            for s_ in range(1, 8):
                P.op("dve", lambda e, s_=s_: e.memset(gbias[:, s_, 24:24 + s_], 0.0), reads=[B_c2], writes=[B_c2])
            for i in range(2):
                P.op("dve", lambda e, i=i: e.memset(stage[i][:], 0.0), writes=[B_stage[i]])
                P.op("dve", lambda e, i=i: e.memset(Vaug[i][:, :, 64:65], 1.0), writes=[B_V[i]])

            def load_head(h):
                i = h % 2
                P.dma("sp", Kaug[i][0:64, :], kT_scr[h, :, :], reads=[B_kscr], writes=[B_K[i]])
                P.dma("sp", Vaug[i][:, :, 0:64], v_scr[:, h * 64:(h + 1) * 64].rearrange("(kt p) c -> p kt c", p=128), reads=[B_vscr], writes=[B_V[i]])

            load_head(0)
            load_head(1)
            pieces = []
            w_in_v = w_in.rearrange("(dc p) f -> p dc f", p=128)
            for i in range(8):
                pieces.append((w_in_v[:, :, i * 64:(i + 1) * 64], wpg[:, :, i * 64:(i + 1) * 64], (8, 64)))
            for i in range(32):
                pieces.append((w_in_v[:, :, 2048 + i * 64:2048 + (i + 1) * 64], wpg[:, :, 512 + i * 64:512 + (i + 1) * 64], (8, 64)))
            pieces.append((pool_w.rearrange("g i o -> i g o"), pw[:, :, :], (4, 128)))
            wbp_v = wbp.rearrange("(g p) f -> p g f", p=128)
            wba_v = wba.rearrange("(g p) f -> p g f", p=128)
            for i in range(8):
                pieces.append((wbp_v[:, :, i * 128:(i + 1) * 128], wbpb[:, :, i * 128:(i + 1) * 128], (4, 128)))
            for i in range(8):
                pieces.append((wba_v[:, :, i * 128:(i + 1) * 128], wbab[:, :, i * 128:(i + 1) * 128], (4, 128)))
            wo_v = w_out.rearrange("(dc p) f -> p dc f", p=128)
            for i in range(16):
                pieces.append((wo_v[:, :, i * 64:(i + 1) * 64], woutb[:, :, i * 64:(i + 1) * 64], (8, 64)))
            stg_state = {"dma": 0, "cast": 0}

            def stg_view(k):
                a, b_ = pieces[k][2]
                return sqr[k % 2][:, 0:a * b_].rearrange("p (a b) -> p a b", a=a)

            def stage_step(ndma):
                while stg_state["cast"] < stg_state["dma"]:
                    k = stg_state["cast"]
                    P.op("dve", lambda e, k=k: e.tensor_copy(pieces[k][1], stg_view(k)), reads=[B_sqr[k % 2]], writes=[B_wc1])
                    stg_state["cast"] += 1
                for _ in range(ndma):
                    k = stg_state["dma"]
                    if k >= len(pieces):
                        break
                    P.dma("sp", stg_view(k), pieces[k][0], writes=[B_sqr[k % 2]])
                    stg_state["dma"] += 1
            for tt in range(16):
                if tt % 3 == 0:
                    stage_step(2)
                s_ = tt // 2
                r = tt % 2
                bk, bb = pb()
                for h in range(8):
                    P.op("pe", lambda e, tt=tt, bk=bk, h=h: e.matmul(bk[:, h * 32:(h + 1) * 32], lhsT=Qaug[0:64, h, tt * 128:(tt + 1) * 128], rhs=kmT[:, h, :],
                                                                     start=True, stop=True), reads=[B_Q[h], B_km], writes=[bb])
                P.op("dve", lambda e, s_=s_, bk=bk, r=r: e.tensor_tensor(gb[r][:], bk[:, 0:256].rearrange("p (h n) -> p h n", h=8),
                                                                         gbias[:, s_, :].unsqueeze(1).to_broadcast([128, 8, 32]), ALU.add),
                     reads=[bb, B_c2], writes=[B_gb[r]])
                for h in range(8):
                    P.op("dve", lambda e, r=r, h=h: e.max(out=mx8[r][:, h, :], in_=gb[r][:, h, :]), reads=[B_gb[r]], writes=[B_mx[r][h]])
                P.op("dve", lambda e, r=r: e.tensor_scalar(thr[r][:], mx8[r][:, :, 2:3], -1e29, None, op0=ALU.max), reads=B_mx[r], writes=[B_thr[r]])
                P.op("dve", lambda e, r=r: e.tensor_tensor(stage[r][:, :, 64:96], gb[r][:], thr[r][:].to_broadcast([128, 8, 32]), ALU.is_lt),
                     reads=[B_gb[r], B_thr[r]], writes=[B_stage[r]])
                P.op("dve", lambda e, r=r, s_=s_: e.memset(stage[r][:, :, 64 + 24 + s_:64 + 25 + s_], 0.0), reads=[B_stage[r]], writes=[B_stage[r]])
                for h in range(8):
                    bk2, bb2 = pb()
                    trv = bk2[:, :].bitcast(BF16)
                    P.op("pe", lambda e, trv=trv, r=r, h=h: e.transpose(trv[0:96, 0:128], stage[r][:, h, :], ident_b[:]), reads=[B_stage[r], B_const], writes=[bb2])
                    P.op("act", lambda e, tt=tt, trv=trv, h=h: e.mul(Qaug[64:96, h, tt * 128:(tt + 1) * 128], trv[64:96, 0:128], NEGM), reads=[bb2], writes=[B_Qm[h]])
            LOOK = 2
            for h in range(8):
                if 1 <= h < 7:
                    load_head(h + 1)
                Kh, bK = Kaug[h % 2], B_K[h % 2]
                Vh, bV = Vaug[h % 2], B_V[h % 2]
                for c in range(4):
                    stage_step(2)
                    oacc, ob = banks[7], bbuf[7]
                    rot["set"] = [6]
                    jobs = [(kt, 0, 512, None) for kt in range(0, 48, 2)]
                    for m in range(2 * c + 2):
                        kt = 48 + 2 * m
                        if m < 2 * c:
                            jobs.append((kt, 0, 512, None))
                        elif m == 2 * c:
                            jobs.append((kt, 0, 512, 0))
                        else:
                            jobs.append((kt, 256, 512, 256))
                    q0 = c * 512
                    nj = len(jobs)
                    pts = [None] * nj
                    for ji in range(nj + LOOK):
                        if ji < nj:
                            kt, c0, c1, dg = jobs[ji]
                            pp = rotn("spair", 3)
                            pr_, pbb = pairs[pp], [bbuf[2 * pp], bbuf[2 * pp + 1]]
                            prv = pr_[:, :].rearrange("p (b n) -> p b n", b=2)
                            for hf in range(2):
                                P.op("pe", lambda e, kt=kt, c0=c0, c1=c1, prv=prv, hf=hf: e.matmul(prv[:, hf, c0:c1], lhsT=Kh[:, (kt + hf) * 128:(kt + hf + 1) * 128],
                                                                                                   rhs=Qaug[:, h, q0 + c0:q0 + c1], start=True, stop=True),
                                     reads=[bK, B_Q[h], B_Qm[h]], writes=[pbb[hf]])
                            pi = rotn("pt", 3)
                            p_, bp = Pt[pi], B_Pt[pi]
                            pts[ji] = (p_, bp)
                            P.op("act", lambda e, c0=c0, c1=c1, prv=prv, p_=p_: e.activation(p_[:, :, c0:c1], prv[:, :, c0:c1], AF.Exp), reads=pbb, writes=[bp])
                            if dg is not None:
                                P.op("dve", lambda e, dg=dg, p_=p_: e.tensor_tensor(p_[:, :, dg:dg + 256], p_[:, :, dg:dg + 256], tri[:, :, :], ALU.mult),
                                     reads=[bp, B_const], writes=[bp])
                        jv = ji - LOOK
                        if jv >= 0:
                            kt, c0, c1, dg = jobs[jv]
                            p_, bp = pts[jv]
                            for hf in range(2):
                                P.op("pe", lambda e, kt=kt, c0=c0, c1=c1, p_=p_, jv=jv, hf=hf: e.matmul(oacc[0:65, c0:c1], lhsT=Vh[:, kt + hf, :], rhs=p_[:, hf, c0:c1],
                                                                                                      start=(jv == 0 and hf == 0), stop=(jv == nj - 1 and hf == 1)),
                                     reads=[bV, bp], writes=[ob])
                    P.op("act", lambda e: e.copy(Osb[:], oacc[0:65, :]), reads=[ob], writes=[B_O])
                    P.op("dve", lambda e: e.reciprocal(Osb[64:65, :], Osb[64:65, :]), reads=[B_O], writes=[B_O])
                    bk, bb = pb()
                    P.op("pe", lambda e, bk=bk: e.matmul(bk[0:64, :], lhsT=ones_f[64:65, 0:64], rhs=Osb[64:65, :], start=True, stop=True),
                         reads=[B_O, B_const], writes=[bb])
                    P.op("dve", lambda e, bk=bk: e.tensor_tensor(Osb[0:64, :], Osb[0:64, :], bk[0:64, :], ALU.mult), reads=[B_O, bb], writes=[B_O])
                    pr = 64 * (h % 2)
                    P.op("act", lambda e, c=c, pr=pr: e.copy(attnT[pr:pr + 64, h // 2, c * 512:(c + 1) * 512], Osb[0:64, :]), reads=[B_O], writes=[B_att])
                    rot["set"] = list(range(8))
            while stg_state["cast"] < len(pieces):
                stage_step(2)
            if debug:
                o = dbg_out("attnT", [128, 4, TOK], BF16)
                P.dma("sp", o, attnT[:], reads=[B_att])
            P.barrier(nopool=True)
        midq.close()
        if stop_after == 2:
            mid.close(); c1w.close(); conv.close()
            return nc, dbg

        B_x1s = Buf()
        x1s_v = x1_scr.rearrange("(dc p) t -> p dc t", p=128)
        with ExitStack() as ph:
            T = lambda name, shape, dt: ph.enter_context(nc.sbuf_tensor(name, list(shape), dt))
            NT = 256
            icn = T("icn", [128, 64], F32)
            B_w = B_wc1
            P.dma("sp", icn[:], invcnt.partition_broadcast(128), writes=[B_w])
            xc2 = [T("xcc%d" % i, [128, 8, NT], F32) for i in range(2)]; B_xc2 = [Buf(), Buf()]
            rstd = T("rstdc", [128, NT], F32); B_rstd = Buf()
            h2_ = [T("hTc%d" % i, [128, 8, NT], BF16) for i in range(2)]; B_h2_ = [Buf(), Buf()]
            ub = T("ub", [128, 4, 16 + NT], F32); B_ub = Buf()
            sw_ = T("sw_", [128, 16 + NT], F32); sx_ = T("sx_", [128, 16 + NT], F32); B_s = Buf()
            dif = T("dif", [128, 4, NT], BF16); B_dif = Buf()
            mixT = T("mixT", [128, 4, NT], BF16); B_mix = Buf()
            sgp = T("sgp", [128, 8, NT], F32); B_sgp = [Buf() for _ in range(8)]
            sga = T("sga", [128, 8, NT], F32); B_sga = [Buf() for _ in range(8)]
            sg = [T("sg%d" % i, [128, NT], F32) for i in range(4)]; B_sg = [Buf() for _ in range(4)]
            mrg = T("mrg", [128, 8, NT], BF16); B_mrg = Buf()
            x1c = T("x1c", [128, 8, NT], F32); B_x1 = Buf()
            xh = T("xh", [128, 8, 16], F32); hh = T("hh", [128, 8, 16], BF16); B_xh = Buf()
            rsh = T("rsh", [128, 16], F32); B_rsh = Buf()
            W = 16 + NT

            P.dma("sp", xh[:], xhT.rearrange("(dc p) t -> p dc t", p=128), writes=[B_xh])
            rms_rstd(lambda dc: xh[:, dc, :], 16, rsh[:], B_xh, B_rsh)
            modulate(lambda dc: xh[:, dc, :], 16, rsh[:], lambda dc: hh[:, dc, :], B_xh, B_rsh, B_xh, SHIFT1, gs1)
            for g in range(4):
                bk, bb = pb()
                for dc in range(8):
                    P.op("pe", lambda e, g=g, dc=dc, bk=bk: e.matmul(bk[:, 0:16], lhsT=wpg[:, dc, g * 128:(g + 1) * 128], rhs=hh[:, dc, :],
                                                                     start=(dc == 0), stop=(dc == 7)), reads=[B_w, B_xh], writes=[bb])
                P.op("dve", lambda e, g=g, bk=bk: e.tensor_scalar(ub[:, g, NT:W], bk[:, 0:16], hflag[:, 0:1], None, op0=ALU.mult),
                     reads=[bb, B_const], writes=[B_ub])

            def norm1(ci):
                c0 = ci * NT
                xc, B_xc = xc2[ci % 2], B_xc2[ci % 2]
                h_, bh = h2_[ci % 2], B_h2_[ci % 2]
                P.dma("sp", xc[:], xT_v[:, :, 6144 + c0:6144 + c0 + NT], writes=[B_xc])
                rms_rstd(lambda dc: xc[:, dc, :], NT, rstd[:], B_xc, B_rstd)
                modulate(lambda dc: xc[:, dc, :], NT, rstd[:], lambda dc: h_[:, dc, :], B_xc, B_rstd, bh, SHIFT1, gs1)

            def rest(ci):
                c0 = ci * NT
                xc, B_xc = xc2[ci % 2], B_xc2[ci % 2]
                h_, bh = h2_[ci % 2], B_h2_[ci % 2]

                def proj_fm(col0, bk):
                    for dc in range(8):
                        P.op("pe", lambda e, dc=dc: e.matmul(bk[0][:, 0:NT], lhsT=wpg[:, dc, col0:col0 + 128], rhs=h_[:, dc, :],
                                                             start=(dc == 0), stop=(dc == 7)), reads=[B_w, bh], writes=[bk[1]])

                P.op("dve", lambda e: e.tensor_copy(ub[:, :, 0:16], ub[:, :, NT:W]), reads=[B_ub], writes=[B_ub])
                for g in range(4):
                    bk = pb(); proj_fm(g * 128, bk)
                    P.op("act", lambda e, g=g, bk=bk: e.copy(ub[:, g, 16:W], bk[0][:, 0:NT]), reads=[bk[1], B_ub], writes=[B_ub])
                for fc in range(8):
                    kg = pb(); proj_fm(512 + fc * 128, kg)
                    P.op("act", lambda e, fc=fc, kg=kg: e.activation(sgp[:, fc, :], kg[0][:, 0:NT], AF.Sigmoid), reads=[kg[1]], writes=[B_sgp[fc]])
                    ka = pb(); proj_fm(1536 + fc * 128, ka)
                    P.op("act", lambda e, fc=fc, ka=ka: e.activation(sga[:, fc, :], ka[0][:, 0:NT], AF.Sigmoid), reads=[ka[1]], writes=[B_sga[fc]])
                for g in range(4):
                    wdw = 2 << g
                    cur = ub[:, g, :]
                    sh = 1
                    for step in range(g + 1):
                        dst = sw_ if step % 2 == 0 else sx_
                        lo = 2 * sh - 1
                        P.op("dve", lambda e, cur=cur, dst=dst, sh=sh, lo=lo: e.tensor_tensor(
                            dst[:, lo:W], cur[:, lo:W], cur[:, lo - sh:W - sh], ALU.add), reads=[B_ub, B_s], writes=[B_s])
                        cur = dst[:, :]
                        sh *= 2
                    P.op("dve", lambda e, g=g, cur=cur, wdw=wdw: e.scalar_tensor_tensor(
                        dif[:, g, :], cur[:, 16:W], 1.0 / wdw, ub[:, g, 16:W], op0=ALU.mult, op1=ALU.subtract),
                        reads=[B_s, B_ub, B_dif], writes=[B_dif])
                    if ci == 0:
                        P.op("dve", lambda e, g=g, cur=cur: e.tensor_tensor(rsh[:], cur[:, 16:32], icn[:, g * 16:(g + 1) * 16], ALU.mult),
                             reads=[B_s, B_w, B_rsh], writes=[B_rsh])
                        P.op("dve", lambda e, g=g: e.tensor_tensor(dif[:, g, 0:16], rsh[:], ub[:, g, 16:32], ALU.subtract),
                             reads=[B_rsh, B_ub, B_dif], writes=[B_dif])
                for g in range(4):
                    bk, bb = pb()
                    P.op("pe", lambda e, g=g, bk=bk: e.matmul(bk[:, 0:NT], lhsT=pw[:, g, :], rhs=dif[:, g, :], start=True, stop=True),
                         reads=[B_w, B_dif], writes=[bb])
                    P.op("dve", lambda e, g=g, bk=bk: e.tensor_scalar(mixT[:, g, :], bk[:, 0:NT], psc[:, g:g + 1], None, op0=ALU.mult),
                         reads=[bb, B_const, B_mix], writes=[B_mix])
                for fc in range(8):
                    kp, kpb = pb()
                    for g in range(4):
                        P.op("pe", lambda e, g=g, fc=fc, kp=kp: e.matmul(kp[:, 0:NT], lhsT=wbpb[:, g, fc * 128:(fc + 1) * 128], rhs=mixT[:, g, :],
                                                                         start=(g == 0), stop=(g == 3)), reads=[B_w, B_mix], writes=[kpb])
                    kq, kqb = pb()
                    for hp in range(4):
                        P.op("pe", lambda e, hp=hp, fc=fc, kq=kq: e.matmul(kq[:, 0:NT], lhsT=wbab[:, hp, fc * 128:(fc + 1) * 128], rhs=attnT[:, hp, c0:c0 + NT],
                                                                           start=(hp == 0), stop=(hp == 3)), reads=[B_w, B_att], writes=[kqb])
                    j = rotn("sg", 4)
                    j2 = rotn("sg", 4)
                    P.op("dve", lambda e, j=j, fc=fc, kp=kp: e.tensor_tensor(sg[j][:], kp[:, 0:NT], sgp[:, fc, :], ALU.mult),
                         reads=[kpb, B_sgp[fc]], writes=[B_sg[j]])
                    P.op("dve", lambda e, j2=j2, fc=fc, kq=kq: e.tensor_tensor(sg[j2][:], kq[:, 0:NT], sga[:, fc, :], ALU.mult),
                         reads=[kqb, B_sga[fc]], writes=[B_sg[j2]])
                    P.op("dve", lambda e, j=j, j2=j2, fc=fc: e.tensor_tensor(mrg[:, fc, :], sg[j2][:], sg[j][:], ALU.add),
                         reads=[B_sg[j], B_sg[j2], B_mrg], writes=[B_mrg])
                for oc in range(8):
                    bk, bb = pb()
                    for fc in range(8):
                        P.op("pe", lambda e, oc=oc, fc=fc, bk=bk: e.matmul(bk[:, 0:NT], lhsT=woutb[:, fc, oc * 128:(oc + 1) * 128], rhs=mrg[:, fc, :],
                                                                           start=(fc == 0), stop=(fc == 7)), reads=[B_w, B_mrg], writes=[bb])
                    P.op("dve", lambda e, oc=oc, bk=bk: e.scalar_tensor_tensor(x1c[:, oc, :], bk[:, 0:NT], GATE1(oc), xc[:, oc, :], op0=ALU.mult, op1=ALU.add),
                         reads=[bb, B_mod, B_xc, B_x1], writes=[B_x1])
                P.dma("sp", x1s_v[:, :, c0:c0 + NT], x1c[:], reads=[B_x1], writes=[B_x1s])

            norm1(0)
            for ci in range(TOK // NT):
                if ci + 1 < TOK // NT:
                    norm1(ci + 1)
                rest(ci)
            if debug:
                o = dbg_out("x1T", [D, TOK])
                P.dma("sp", o, x1_scr, reads=[B_x1s])
            P.barrier(nopool=True)
        mid.close()
        c1w.close()
        if stop_after == 3:
            conv.close()
            return nc, dbg

        late = ExitStack()
        TL = lambda name, shape, dt: late.enter_context(nc.sbuf_tensor(name, list(shape), dt))
        h2T = TL("h2T", [128, 8, TOK], BF16); B_h2 = Buf()
        iT = TL("iT", [128, TOK], F32); jT = TL("jT", [128, TOK], F32); gT = TL("gT", [128, TOK], F32)
        B_ijg = Buf()
        with ExitStack() as ph:
            T = lambda name, shape, dt: ph.enter_context(nc.sbuf_tensor(name, list(shape), dt))
            wqb = T("wqb", [128, 8, 2048], BF16)
            skb = T("skb", [128, 16, 128], BF16)
            B_w = Buf()
            stg_scope = ExitStack()
            x1c = stg_scope.enter_context(nc.sbuf_tensor("stg1", [128, 8, 512], F32)); B_x1 = Buf()
            stg2 = stg_scope.enter_context(nc.sbuf_tensor("stg2", [128, 8, 512], F32)); B_stg2 = Buf()
            pieces = []
            wq_v = wq.rearrange("(dc p) f -> p dc f", p=128)
            for i in range(4):
                pieces.append((wq_v[:, :, i * 512:(i + 1) * 512], wqb[:, :, i * 512:(i + 1) * 512], (8, 512)))
            for i in range(4):
                pieces.append((skT[:, i * 4:(i + 1) * 4, :], skb[:, i * 4:(i + 1) * 4, :], (4, 128)))
            stgs = [(x1c, B_x1), (stg2, B_stg2)]
            for i, (src, dst, (a, b_)) in enumerate(pieces):
                st_, bs = stgs[i % 2]
                sv = st_[:, 0:a, 0:b_]
                P.dma("sp", sv, src, writes=[bs])
                if i % 2 == 0:
                    P.op("dve", lambda e, dst=dst, sv=sv: e.tensor_copy(dst, sv), reads=[bs], writes=[B_w])
                else:
                    P.op("act", lambda e, dst=dst, sv=sv: e.copy(dst, sv), reads=[bs], writes=[B_w])
            P.barrier(nopool=True)
            stg_scope.close()
            NQ = 256
            x1c2 = [T("x1e%d" % i, [128, 8, NQ], F32) for i in range(2)]; B_x12 = [Buf(), Buf()]
            rstd = T("rstde", [128, NQ], F32); B_rstd = Buf()
            qT2 = [T("qT%d" % i, [128, 16, NQ], BF16) for i in range(2)]; B_qT2 = [Buf(), Buf()]
            ssb = [T("ssb%d" % i, [128, 16, 128], F32) for i in range(2)]; B_ssb = [[Buf() for _ in range(16)] for _ in range(2)]
            wk16 = T("wk16", [128, 16, 128], F32); B_wk16 = [Buf() for _ in range(16)]
            wk8 = wk16[:].rearrange("p (h two) k -> p h (two k)", two=2)
            sv_ = T("sv_", [128, 16, 16], F32); sidx = T("sidx", [128, 16, 16], U32); sidf = T("sidf", [128, 16, 16], F32)
            B_sva = [Buf() for _ in range(16)]; B_svb = [Buf() for _ in range(16)]
            B_ixa = [Buf() for _ in range(16)]; B_ixb = [Buf() for _ in range(16)]; B_sidf = Buf()
            cand = T("cand", [128, 8, 256], F32); B_cand = [Buf() for _ in range(8)]
            cv = T("cv", [128, 8, 16], F32); cpos = T("cpos", [128, 8, 16], U32); cpf = T("cpf", [128, 8, 16], F32)
            B_cva = [Buf() for _ in range(8)]; B_cvb = [Buf() for _ in range(8)]
            B_cpa = [Buf() for _ in range(8)]; B_cpb = [Buf() for _ in range(8)]; B_cpf = Buf()
            ak = T("ak", [128, 8, 16], F32); bk_ = T("bk_", [128, 8, 16], F32); B_ab = Buf()
            big = T("big", [128, 8, 16, 16], F32); B_big = Buf()
            bigi = T("bigi", [128, 8, 16, 16], F32); B_bigi = Buf()
            bigj = big; B_bigj = B_big
            iK = T("iK", [128, 8, 16], F32); jK = T("jK", [128, 8, 16], F32); gK = T("gK", [128, 8, 16], F32)
            B_iK = Buf(); B_jK = Buf(); B_gK = Buf()
            ssum = T("ssum", [128, 8], F32); B_ss = Buf()
            def front(ci):
                c0 = ci * NQ
                x1c, B_x1 = x1c2[ci % 2], B_x12[ci % 2]
                qT, B_qT = qT2[ci % 2], B_qT2[ci % 2]
                P.dma("sp", x1c[:], x1s_v[:, :, c0:c0 + NQ], reads=[B_x1s], writes=[B_x1])
                rms_rstd(lambda dc: x1c[:, dc, :], NQ, rstd[:], B_x1, B_rstd)
                modulate(lambda dc: x1c[:, dc, :], NQ, rstd[:], lambda dc: h2T[:, dc, c0:c0 + NQ], B_x1, B_rstd, B_h2, SHIFT2, gs2)
                for hp in range(16):
                    bk, bb = pb()
                    for dc in range(8):
                        P.op("pe", lambda e, hp=hp, dc=dc, bk=bk: e.matmul(bk[:, 0:NQ], lhsT=wqb[:, dc, hp * 128:(hp + 1) * 128], rhs=h2T[:, dc, c0:c0 + NQ],
                                                                           start=(dc == 0), stop=(dc == 7)), reads=[B_w, B_h2], writes=[bb])
                    P.op("act", lambda e, hp=hp, bk=bk: e.copy(qT[:, hp, :], bk[:, 0:NQ]), reads=[bb, B_qT], writes=[B_qT])

            front(0)
            for ci in range(TOK // NQ):
                c0 = ci * NQ
                if ci + 1 < TOK // NQ:
                    front(ci + 1)
                qT, B_qT = qT2[ci % 2], B_qT2[ci % 2]
                for tt in range(NQ // 128):
                    t0 = c0 + tt * 128
                    rs = rotn("ssb")
                    ss_, bss = ssb[rs], B_ssb[rs]
                    for g4 in range(4):
                        bk, bb = pb()
                        for q in range(4):
                            hp = g4 * 4 + q
                            P.op("pe", lambda e, hp=hp, q=q, bk=bk: e.matmul(bk[:, q * 128:(q + 1) * 128], lhsT=qT[:, hp, tt * 128:(tt + 1) * 128], rhs=skb[:, hp, :],
                                                                             start=True, stop=True), reads=[B_qT, B_w], writes=[bb])
                        P.op("act", lambda e, g4=g4, bk=bk, ss_=ss_: e.copy(ss_[:, g4 * 4:(g4 + 1) * 4, :], bk[:, :].rearrange("p (a b) -> p a b", a=4)),
                             reads=[bb], writes=bss[g4 * 4:(g4 + 1) * 4])
                    R16 = range(16)
                    for hp in R16:
                        P.op("dve", lambda e, hp=hp: e.max(out=sv_[:, hp, 0:8], in_=ss_[:, hp, :]), reads=[bss[hp]], writes=[B_sva[hp]])
                    for hp in R16:
                        P.op("dve", lambda e, hp=hp: e.max_index(out=sidx[:, hp, 0:8], in_max=sv_[:, hp, 0:8], in_values=ss_[:, hp, :]), reads=[bss[hp], B_sva[hp]], writes=[B_ixa[hp]])
                    for hp in R16:
                        P.op("dve", lambda e, hp=hp: e.match_replace(out=wk16[:, hp, :], in_to_replace=sv_[:, hp, 0:8], in_values=ss_[:, hp, :], imm_value=-1e30),
                             reads=[bss[hp], B_sva[hp]], writes=[B_wk16[hp]])
                    for hp in R16:
                        P.op("dve", lambda e, hp=hp: e.max(out=sv_[:, hp, 8:16], in_=wk16[:, hp, :]), reads=[B_wk16[hp]], writes=[B_svb[hp]])
                    for hp in R16:
                        P.op("dve", lambda e, hp=hp: e.max_index(out=sidx[:, hp, 8:16], in_max=sv_[:, hp, 8:16], in_values=wk16[:, hp, :]), reads=[B_wk16[hp], B_svb[hp]], writes=[B_ixb[hp]])
                    P.op("dve", lambda e: e.tensor_copy(sidf[:], sidx[:]), reads=B_ixa + B_ixb, writes=[B_sidf])
                    svv = sv_[:].rearrange("p (h two) k -> p h two k", two=2)
                    sfv = sidf[:].rearrange("p (h two) k -> p h two k", two=2)
                    candv = cand[:].rearrange("p h (a b) -> p h a b", a=16)
                    P.op("dve", lambda e: e.tensor_tensor(candv, svv[:, :, 0, :].unsqueeze(3).to_broadcast([128, 8, 16, 16]),
                                                          svv[:, :, 1, :].unsqueeze(2).to_broadcast([128, 8, 16, 16]), ALU.add), reads=B_sva + B_svb, writes=B_cand)
                    R8 = range(8)
                    for h in R8:
                        P.op("dve", lambda e, h=h: e.max(out=cv[:, h, 0:8], in_=cand[:, h, :]), reads=[B_cand[h]], writes=[B_cva[h]])
                    for h in R8:
                        P.op("dve", lambda e, h=h: e.max_index(out=cpos[:, h, 0:8], in_max=cv[:, h, 0:8], in_values=cand[:, h, :]), reads=[B_cand[h], B_cva[h]], writes=[B_cpa[h]])
                    for h in R8:
                        P.op("dve", lambda e, h=h: e.match_replace(out=wk8[:, h, :], in_to_replace=cv[:, h, 0:8], in_values=cand[:, h, :], imm_value=-1e30),
                             reads=[B_cand[h], B_cva[h]], writes=[B_wk16[2 * h], B_wk16[2 * h + 1]])
                    for h in R8:
                        P.op("dve", lambda e, h=h: e.max(out=cv[:, h, 8:16], in_=wk8[:, h, :]), reads=[B_wk16[2 * h], B_wk16[2 * h + 1]], writes=[B_cvb[h]])
                    for h in R8:
                        P.op("dve", lambda e, h=h: e.max_index(out=cpos[:, h, 8:16], in_max=cv[:, h, 8:16], in_values=wk8[:, h, :]), reads=[B_wk16[2 * h], B_wk16[2 * h + 1], B_cvb[h]], writes=[B_cpb[h]])
                    P.op("dve", lambda e: e.tensor_copy(cpf[:], cpos[:]), reads=B_cpa + B_cpb, writes=[B_cpf])
                    P.op("dve", lambda e: e.tensor_tensor(gK[:], cv[:], cv[:, :, 0:1].to_broadcast([128, 8, 16]), ALU.subtract), reads=B_cva + B_cvb + [B_gK], writes=[B_gK])
                    P.op("act", lambda e: e.activation(gK[:], gK[:], AF.Exp), reads=[B_gK], writes=[B_gK])
                    th4 = thr16[:].unsqueeze(1).unsqueeze(1).to_broadcast([128, 8, 16, 16])
                    io4 = iota_f[:, 0:16].unsqueeze(1).unsqueeze(1).to_broadcast([128, 8, 16, 16])
                    P.op("dve", lambda e: e.tensor_tensor(big[:], cpf[:].unsqueeze(3).to_broadcast([128, 8, 16, 16]), th4, ALU.is_ge),
                         reads=[B_cpf, B_const, B_big], writes=[B_big])
                    P.op("dve", lambda e: e.tensor_reduce(ak[:], big[:], AX.X, ALU.add), reads=[B_big, B_ab], writes=[B_ab])
                    P.op("dve", lambda e: e.tensor_tensor(bigi[:], io4, ak[:].unsqueeze(3).to_broadcast([128, 8, 16, 16]), ALU.is_equal),
                         reads=[B_ab, B_const, B_bigi], writes=[B_bigi])
                    P.op("dve", lambda e: e.tensor_tensor(bigi[:], bigi[:], sfv[:, :, 0, :].unsqueeze(2).to_broadcast([128, 8, 16, 16]), ALU.mult),
                         reads=[B_bigi, B_sidf], writes=[B_bigi])
                    P.op("dve", lambda e: e.scalar_tensor_tensor(bk_[:], ak[:], -16.0, cpf[:], op0=ALU.mult, op1=ALU.add), reads=[B_cpf, B_ab], writes=[B_ab])
                    P.op("dve", lambda e: e.tensor_tensor(bigj[:], io4, bk_[:].unsqueeze(3).to_broadcast([128, 8, 16, 16]), ALU.is_equal),
                         reads=[B_ab, B_const, B_bigj], writes=[B_bigj])
                    P.op("dve", lambda e: e.tensor_tensor(bigj[:], bigj[:], sfv[:, :, 1, :].unsqueeze(2).to_broadcast([128, 8, 16, 16]), ALU.mult),
                         reads=[B_bigj, B_sidf], writes=[B_bigj])
                    P.op("dve", lambda e: e.tensor_reduce(ssum[:], gK[:], AX.X, ALU.add), reads=[B_gK, B_ss], writes=[B_ss])
                    P.op("dve", lambda e: e.reciprocal(ssum[:], ssum[:]), reads=[B_ss], writes=[B_ss])
                    P.op("dve", lambda e: e.tensor_tensor(gK[:], gK[:], ssum[:].unsqueeze(2).to_broadcast([128, 8, 16]), ALU.mult), reads=[B_gK, B_ss], writes=[B_gK])
                    P.op("dve", lambda e: e.tensor_reduce(iK[:], bigi[:], AX.X, ALU.add), reads=[B_bigi, B_iK], writes=[B_iK])
                    P.op("dve", lambda e: e.tensor_reduce(jK[:], bigj[:], AX.X, ALU.add), reads=[B_bigj, B_jK], writes=[B_jK])
                    for (src, dst, bsrc) in ((gK, gT, B_gK), (iK, iT, B_iK), (jK, jT, B_jK)):
                        bk, bb = pb()
                        P.op("pe", lambda e, src=src, bk=bk: e.transpose(bk[:, 0:128], src[:].rearrange("p h k -> p (h k)"), ident_f[:]), reads=[bsrc, B_const], writes=[bb])
                        P.op("act", lambda e, dst=dst, bk=bk: e.copy(dst[:, t0:t0 + 128], bk[:, 0:128]), reads=[bb], writes=[B_ijg])
            if debug:
                for name, t_ in (("iT", iT), ("jT", jT), ("gT", gT)):
                    o = dbg_out(name, [128, TOK])
                    P.dma("sp", o, t_[:], reads=[B_ijg])
                o = dbg_out("h2T", [128, 8, TOK], BF16)
                P.dma("sp", o, h2T[:], reads=[B_h2])
            P.barrier()
        conv.close()
        if stop_after == 4:
            late.close()
            return nc, dbg

        with ExitStack() as ph:
            T = lambda name, shape, dt: ph.enter_context(nc.sbuf_tensor(name, list(shape), dt))
            TC = 256
            GI = 4
            TB = 16
            NBUF = 3
            Wt = [T("Wt%d" % i, [128, TC, 64], BF16) for i in range(2)]; B_Wt = [Buf(), Buf()]
            ohi = [T("ohi%d" % i, [128, TB, 64], BF16) for i in range(2)]; B_ohi = [Buf(), Buf()]
            ohj = [T("ohj%d" % i, [128, TB, 128], BF16) for i in range(2)]; B_ohj = [Buf(), Buf()]
            ub_ = [T("ub_%d" % i, [128, 8, GI * 128], BF16) for i in range(NBUF)]; B_u = [Buf() for _ in range(NBUF)]
            vb_ = [T("vb_%d" % i, [128, GI, 1024], BF16) for i in range(NBUF)]; B_v = [Buf() for _ in range(NBUF)]
            Gt = [T("Gt%d" % i, [128, TC], BF16) for i in range(2)]; B_G = [Buf(), Buf()]
            WA = [T("WA%d" % i, [128, TC], BF16) for i in range(2)]; B_WA = [Buf(), Buf()]
            x1c = T("x1d", [128, 8, TC], F32); B_x1 = Buf()
            rstd = T("rstdd", [128, TC], F32); B_rstd = Buf()
            outT_v = outT.rearrange("(dc p) t -> p dc t", p=128)
            NG = 128 // GI
            NCHK = TOK // TC
            gcount = {"n": 0}

            def load_tables(gidx):
                ig = gidx % NG
                b_ = gidx % NBUF
                P.dma("sp", ub_[b_][:], uT_scr[ig], reads=[B_uTs], writes=[B_u[b_]])
                P.dma("sp", vb_[b_][:], v2_scr[ig], reads=[B_v2s], writes=[B_v[b_]])

            def wbuild_steps(ch, half, buf):
                tbase = ch * TC
                io_i = iota_b[:, half * 64:(half + 1) * 64].unsqueeze(1).to_broadcast([128, TB, 64])
                io_j = iota_b[:].unsqueeze(1).to_broadcast([128, TB, 128])

                def onehots(tb):
                    tg = tbase + tb * TB
                    r = tb % 2
                    P.op("dve", lambda e: e.tensor_tensor(ohj[r][:], io_j, jT[:, tg:tg + TB].unsqueeze(2).to_broadcast([128, TB, 128]), ALU.is_equal),
                         reads=[B_const, B_ijg], writes=[B_ohj[r]])
                    P.op("dve", lambda e: e.tensor_tensor(ohi[r][:], io_i, iT[:, tg:tg + TB].unsqueeze(2).to_broadcast([128, TB, 64]), ALU.is_equal),
                         reads=[B_const, B_ijg], writes=[B_ohi[r]])
                    P.op("dve", lambda e: e.tensor_tensor(ohi[r][:], ohi[r][:], gT[:, tg:tg + TB].unsqueeze(2).to_broadcast([128, TB, 64]), ALU.mult),
                         reads=[B_ohi[r], B_ijg], writes=[B_ohi[r]])

                onehots(0)
                yield
                for tb in range(TC // TB):
                    if tb + 1 < TC // TB:
                        onehots(tb + 1)
                    r = tb % 2
                    for q8 in range(TB // 8):
                        t8 = tb * (TB // 8) + q8
                        wbk, wbb = banks[6 + t8 % 2], bbuf[6 + t8 % 2]
                        for q in range(8):
                            tok = q8 * 8 + q
                            P.op("pe", lambda e, q=q, tok=tok, wbk=wbk: e.matmul(wbk[:, q * 64:(q + 1) * 64], lhsT=ohj[r][:, tok, :], rhs=ohi[r][:, tok, :],
                                                                                 start=True, stop=True), reads=[B_ohi[r], B_ohj[r]], writes=[wbb])
                        P.op("act", lambda e, t8=t8, wbk=wbk: e.copy(Wt[buf][:, t8 * 8:(t8 + 1) * 8, :], wbk[:, :].rearrange("p (t i) -> p t i", t=8)),
                             reads=[wbb, B_Wt[buf]], writes=[B_Wt[buf]])
                    yield

            sched = [(ch, half) for ch in range(NCHK) for half in range(2)]
            for _ in wbuild_steps(0, 0, 0):
                pass
            for g_ in range(NBUF):
                load_tables(g_)
            for k, (ch, half) in enumerate(sched):
                t0 = ch * TC
                gen = wbuild_steps(sched[k + 1][0], sched[k + 1][1], (k + 1) % 2) if k + 1 < len(sched) else None
                wt_, bwt = Wt[k % 2], B_Wt[k % 2]
                if half == 1:
                    P.dma("sp", x1c[:], x1s_v[:, :, t0:t0 + TC], reads=[B_x1s], writes=[B_x1])
                pend = None
                for il in range(64 + 1):
                    i = half * 64 + il
                    if il < 64:
                        gidx = gcount["n"] + i // GI
                        b_ = gidx % NBUF
                        u_, bu = ub_[b_], B_u[b_]
                        ii = i % GI
                        abk, abb = banks[4 + i % 2], bbuf[4 + i % 2]
                        for dc in range(8):
                            P.op("pe", lambda e, dc=dc, ii=ii, abk=abk, u_=u_: e.matmul(abk[:, 0:TC], lhsT=u_[:, dc, ii * 128:(ii + 1) * 128], rhs=h2T[:, dc, t0:t0 + TC],
                                                                                        start=(dc == 0), stop=(dc == 7)), reads=[bu, B_h2], writes=[abb])
                        g_, bg = Gt[i % 2], B_G[i % 2]
                        w_, bw = WA[i % 2], B_WA[i % 2]
                        P.op("act", lambda e, abk=abk, g_=g_: e.activation(g_[:], abk[:, 0:TC], AF.Gelu), reads=[abb], writes=[bg])
                        P.op("pool", lambda e, il=il, g_=g_, w_=w_, wt_=wt_: e.tensor_tensor(w_[:], g_[:], wt_[:, :, il], ALU.mult), reads=[bg, bwt], writes=[bw])
                    if pend is not None:
                        pi_, pw_, pbw, pg = pend
                        v_, bv = vb_[pg % NBUF], B_v[pg % NBUF]
                        pil = pi_ % GI
                        for oc in range(8):
                            P.op("pe", lambda e, oc=oc, pil=pil, pi_=pi_, pw_=pw_, v_=v_: e.matmul(
                                banks[oc // 2][:, (oc % 2) * TC:(oc % 2) * TC + TC], lhsT=v_[:, pil, oc * 128:(oc + 1) * 128], rhs=pw_[:],
                                start=(pi_ == 0 and oc % 2 == 0), stop=(pi_ == 127), skip_group_check=True), reads=[bv, pbw], writes=[bbuf[oc // 2]])
                        if pi_ % GI == GI - 1:
                            nxt = pg + NBUF
                            if nxt < NCHK * NG:
                                load_tables(nxt)
                    pend = (i, w_, bw, gidx) if il < 64 else None
                    if gen is not None and il < 64 and il % 4 == 3:
                        next(gen, None)
                if gen is not None:
                    for _ in gen:
                        pass
                if half == 0:
                    continue
                gcount["n"] += NG
                for oc in range(8):
                    P.op("dve", lambda e, oc=oc: e.scalar_tensor_tensor(x1c[:, oc, :], banks[oc // 2][:, (oc % 2) * TC:(oc % 2) * TC + TC], GATE2(oc), x1c[:, oc, :],
                                                                        op0=ALU.mult, op1=ALU.add), reads=[bbuf[oc // 2], B_mod, B_x1], writes=[B_x1])
                rot["set"] = [6, 7]
                rms_rstd(lambda dc: x1c[:, dc, :], TC, rstd[:], B_x1, B_rstd)
                rot["set"] = list(range(8))
                for dc in range(8):
                    P.op("dve", lambda e, dc=dc: e.scalar_tensor_tensor(x1c[:, dc, :], x1c[:, dc, :], gfin[:, dc:dc + 1], rstd[:], op0=ALU.mult, op1=ALU.mult),
                         reads=[B_x1, B_rstd, B_const], writes=[B_x1])
                P.dma("sp", outT_v[:, :, t0:t0 + TC], x1c[:], reads=[B_x1])
            P.barrier()
        late.close()
    return nc, dbg


def make_inputs(x, c, w_ada, b_ada, norm_mix_g, w_in, pool_w, pool_scale, w_branch_pool, w_branch_attn, w_out,
                norm_ffn_g, peer_wq, peer_sub_keys, peer_u, peer_v, norm_final_g):
    f = lambda a: np.ascontiguousarray(np.asarray(a, dtype=np.float32))
    x = f(x); c = f(c)
    colT = lambda v, k: f(np.asarray(v, np.float32).reshape(k, 128).T)
    w_in0 = f(w_in[0])
    perm = []
    for base in (512, 1024):
        for h in range(8):
            o = base + 64 * h
            perm += list(range(o + 8, o + 16)) + list(range(o, o + 8)) + list(range(o + 16, o + 64))
    w_sw = f(w_in0[:, perm])
    shared = dict(
        w_ada=f(w_ada[0]), w_in=w_in0, w_sw=w_sw, pool_w=f(pool_w[0]), wbp=f(w_branch_pool[0]), wba=f(w_branch_attn[0]),
        w_out=f(w_out[0]), wq=f(peer_wq[0]),
        skT=f(np.asarray(peer_sub_keys[0], np.float32).reshape(16, 128, 128).transpose(2, 0, 1)),
        uT=f(np.asarray(peer_u[0], np.float32).T), vtab=f(peer_v[0]),
        badaT=colT(b_ada[0], 48), gmixT=colT(norm_mix_g[0], 8), gffnT=colT(norm_ffn_g[0], 8), gfinT=colT(norm_final_g, 8),
        pscT=colT(pool_scale[0], 4),
    )
    half = 8
    inv = (500000.0 ** (-np.arange(half, dtype=np.float32) / half)).astype(np.float32)
    in_maps = []
    for core in range(8):
        b, j = core // 4, core % 4
        own = list(range(8 * j, 8 * j + 8))
        others = [n for n in range(32) if n not in own]
        slots = others + own
        tok = np.concatenate([np.arange(n * 256, (n + 1) * 256) for n in slots])
        xT = f(x[b].T[:, tok])
        xh = np.zeros((D, 16), np.float32)
        if j > 0:
            xh = f(x[b, 2048 * j - 16:2048 * j].T)
        ang = tok.astype(np.float32)[None, :] * inv[:, None]
        cosv = np.cos(ang).astype(np.float32); sinv = np.sin(ang).astype(np.float32)
        cF = np.ones((64, S), np.float32); sF = np.zeros((64, S), np.float32)
        cF[0:8] = cosv; cF[8:16] = cosv; sF[0:8] = -sinv; sF[8:16] = sinv
        pastb = np.array([[0.0 if n < 8 * j else -1e30 for n in others]], np.float32)
        ic = np.zeros((4, 16), np.float32)
        for g in range(4):
            w = 2 << g
            for t in range(16):
                ic[g, t] = 1.0 / min(t + 1, w) if j == 0 else 1.0 / w
        m = dict(shared)
        m.update(xT=xT, xhT=xh, cosF=f(np.concatenate([cF, cF], 0)), sinF=f(np.concatenate([sF, sF], 0)),
                 cT=colT(c[b], 8), pastb=pastb, invcnt=f(ic.reshape(1, 64)),
                 haloflag=np.array([[0.0 if j == 0 else 1.0]], np.float32))
        in_maps.append(m)
    return in_maps


_CACHE = {}


def kernel(**inputs):
    in_maps = make_inputs(**inputs)
    if "nc" not in _CACHE:
        _CACHE["nc"] = build()[0]
    res = run_bass_kernel_spmd(_CACHE["nc"], in_maps, core_ids=list(range(8)))
    out = np.zeros((2, S, D), np.float32)
    for core in range(8):
        b, j = core // 4, core % 4
        out[b, 2048 * j:2048 * (j + 1), :] = res.results[core]["outT"].T
    return out
```

```python
import numpy as np
from contextlib import ExitStack
import concourse.bass as bass
import concourse.mybir as mybir
from concourse.bass_utils import run_bass_kernel_spmd

F32 = mybir.dt.float32
BF16 = mybir.dt.bfloat16
U32 = mybir.dt.uint32
ALU = mybir.AluOpType
AF = mybir.ActivationFunctionType
AX = mybir.AxisListType

D = 1024
S = 8192
TOK = 2048
NCH = 16
EPS = 1e-6
NEGM = -30000.0


class Buf:
    __slots__ = ("w", "r")

    def __init__(self):
        self.w = None
        self.r = {}


class Prog:
    ENG = ("pe", "act", "dve", "pool", "sp")

    def __init__(self, nc, stack, lanes_sp=12, lanes_pool=8, lanes_act=2):
        self.nc = nc
        self.eng = {"pe": nc.tensor, "act": nc.scalar, "dve": nc.vector, "pool": nc.gpsimd, "sp": nc.sync}
        self.sem = {}
        self.cnt = {}
        for e in self.ENG:
            self.sem[e] = stack.enter_context(nc.semaphore("s_" + e))
            self.cnt[e] = 0
        self.lanes = {"sp": [], "pool": [], "act": []}
        for q, n in (("sp", lanes_sp), ("pool", lanes_pool), ("act", lanes_act)):
            for l in range(n):
                s = "L%s%d" % (q, l)
                self.sem[s] = stack.enter_context(nc.semaphore("s_" + s))
                self.cnt[s] = 0
                self.lanes[q].append(s)
        self.rr = {"sp": 0, "pool": 0, "act": 0}
        self.waited = {e: {} for e in self.ENG}

    def _deps(self, reads, writes):
        deps = {}

        def add(s, v):
            if deps.get(s, 0) < v:
                deps[s] = v

        for b in reads:
            if b.w is not None:
                add(*b.w)
        for b in writes:
            if b.w is not None:
                add(*b.w)
            for s, v in b.r.items():
                add(s, v)
        return deps

    def _wait(self, e, deps):
        E = self.eng[e]
        for s, v in deps.items():
            if e == "pe" and s == "pe":
                continue
            if self.waited[e].get(s, 0) >= v:
                continue
            if s == e:
                v = max(v, self.cnt[e] - 2)
            E.wait_ge(self.sem[s], v)
            self.waited[e][s] = v

    def op(self, e, fn, reads=(), writes=()):
        self._wait(e, self._deps(reads, writes))
        ins = fn(self.eng[e])
        self.cnt[e] += 1
        ins.then_inc(self.sem[e], 1)
        c = self.cnt[e]
        for b in reads:
            b.r[e] = c
        for b in writes:
            b.w = (e, c)
            b.r = {}

    def dma(self, q, out, in_, reads=(), writes=()):
        ls = self.lanes[q]
        s = ls[self.rr[q]]
        self.rr[q] = (self.rr[q] + 1) % len(ls)
        deps = self._deps(reads, writes)
        if self.cnt[s] > 0 and deps.get(s, 0) < self.cnt[s]:
            deps[s] = self.cnt[s]
        self._wait(q, deps)
        ins = self.eng[q].dma_start(out=out, in_=in_)
        self.cnt[s] += 16
        ins.then_inc(self.sem[s], 16)
        c = self.cnt[s]
        for b in reads:
            b.r[s] = c
        for b in writes:
            b.w = (s, c)
            b.r = {}

    def barrier(self, nopool=False):
        for e in self.ENG:
            if nopool and e == "pool":
                continue
            deps = {s: v for s, v in self.cnt.items() if v > 0 and s != e
                    and not (nopool and (s == "pool" or s.startswith("Lpool")))}
            self._wait(e, deps)


def build(stop_after=None, debug=False):
    nc = bass.Bass("TRN2", target_bir_lowering=False)
    di = lambda name, shape, dt=F32: nc.dram_tensor(name, list(shape), dt, kind="ExternalInput").ap()
    xT = di("xT", [D, S]); xhT = di("xhT", [D, 16])
    cosF = di("cosF", [128, S]); sinF = di("sinF", [128, S])
    cT = di("cT", [128, 8]); badaT = di("badaT", [128, 48])
    gmixT = di("gmixT", [128, 8]); gffnT = di("gffnT", [128, 8]); gfinT = di("gfinT", [128, 8])
    pscT = di("pscT", [128, 4])
    w_ada = di("w_ada", [D, 6 * D]); w_in = di("w_in", [D, 4096]); w_sw = di("w_sw", [D, 1024])
    pool_w = di("pool_w", [4, 128, 128]); wbp = di("wbp", [512, D]); wba = di("wba", [512, D])
    w_out = di("w_out", [D, D]); wq = di("wq", [D, 2048]); skT = di("skT", [128, 16, 128])
    uT = di("uT", [D, 16384]); vtab = di("vtab", [16384, D])
    pastb = di("pastb", [1, 24]); invcnt = di("invcnt", [1, 64]); haloflag = di("haloflag", [1, 1])
    outT = nc.dram_tensor("outT", [D, TOK], F32, kind="ExternalOutput").ap()
    dscr = lambda name, shape, dt: nc.dram_tensor(name, list(shape), dt, kind="Internal").ap()
    kT_scr = dscr("kT_scr", [8, 64, S], BF16)
    v_scr = dscr("v_scr", [S, 512], BF16)
    x1_scr = dscr("x1_scr", [D, TOK], F32)
    uT_scr = dscr("uT_scr", [32, 128, 8, 512], BF16)
    v2_scr = dscr("v2_scr", [32, 128, 4, 1024], BF16)
    dbg = {}

    def dbg_out(name, shape, dt=F32):
        dbg[name] = nc.dram_tensor("dbg_" + name, list(shape), dt, kind="ExternalOutput").ap()
        return dbg[name]

    with ExitStack() as top:
        P = Prog(nc, top)
        T0 = lambda name, shape, dt: top.enter_context(nc.sbuf_tensor(name, list(shape), dt))
        pairs = [top.enter_context(nc.psum_tensor("pair%d" % i, [128, 1024], F32)) for i in range(4)]
        banks = [pairs[i // 2][:, (i % 2) * 512:(i % 2 + 1) * 512] for i in range(8)]
        bbuf = [Buf() for _ in range(8)]
        rot = {"i": 0, "set": list(range(8))}
        rcnt = {}

        def pb():
            s = rot["set"]
            rot["i"] = (rot["i"] + 1) % len(s)
            k = s[rot["i"]]
            return banks[k], bbuf[k]

        def rotn(name, n=2):
            rcnt[name] = (rcnt.get(name, -1) + 1) % n
            return rcnt[name]

        ones_f = T0("ones_f", [128, 128], F32)
        ident_f = T0("ident_f", [128, 128], F32)
        ident_b = T0("ident_b", [128, 128], BF16)
        iota_f = T0("iota_f", [128, 128], F32)
        iota_b = T0("iota_b", [128, 128], BF16)
        pidx64 = T0("pidx64", [128, 1], F32)
        tri = T0("tri", [128, 2, 256], BF16)
        modT = T0("modT", [128, 48], F32)
        gs1 = T0("gs1", [128, 8], F32); gs2 = T0("gs2", [128, 8], F32)
        gfin = T0("gfin", [128, 8], F32)
        psc = T0("psc", [128, 4], F32)
        hflag = T0("hflag", [128, 1], F32)
        epsT = T0("epsT", [128, 1], F32)
        sqr = [T0("sqr%d" % i, [128, 512], F32) for i in range(2)]
        B_sqr = [Buf(), Buf()]
        sqb = [T0("sqb%d" % i, [128, 512], BF16) for i in range(2)]
        B_sqb = [Buf(), Buf()]
        ones_b = T0("ones_b", [128, 128], BF16)
        B_const = Buf(); B_mod = Buf()

        P.op("dve", lambda e: e.memset(ones_f[:], 1.0), writes=[B_const])
        P.op("dve", lambda e: e.memset(ones_b[:], 1.0), writes=[B_const])
        P.op("dve", lambda e: e.memset(epsT[:], EPS), writes=[B_const])
        P.op("pool", lambda e: e.memset(ident_f[:], 0.0), writes=[B_const])
        P.op("pool", lambda e: e.affine_select(ident_f[:], ident_f[:], pattern=[[-1, 128]], compare_op=ALU.not_equal,
                                               fill=1.0, base=0, channel_multiplier=1), reads=[B_const], writes=[B_const])
        P.op("pool", lambda e: e.iota(iota_f[:], pattern=[[1, 128]], base=0, channel_multiplier=0,
                                      allow_small_or_imprecise_dtypes=True), writes=[B_const])
        P.op("pool", lambda e: e.iota(pidx64[:], pattern=[[0, 1]], base=-64, channel_multiplier=1,
                                      allow_small_or_imprecise_dtypes=True), writes=[B_const])
        P.op("pool", lambda e: e.memset(tri[:], 1.0), writes=[B_const])
        for half in range(2):
            P.op("pool", lambda e, half=half: e.affine_select(tri[:, half, :], tri[:, half, :], pattern=[[1, 256]], compare_op=ALU.is_ge,
                                                              fill=0.0, base=-128 * half, channel_multiplier=-1), reads=[B_const], writes=[B_const])
        P.op("dve", lambda e: e.tensor_copy(ident_b[:], ident_f[:]), reads=[B_const], writes=[B_const])
        P.op("dve", lambda e: e.tensor_copy(iota_b[:], iota_f[:]), reads=[B_const], writes=[B_const])
        thr16 = T0("thr16", [128, 16], F32)
        P.op("dve", lambda e: e.tensor_scalar(thr16[:], iota_f[:, 0:16], 16.0, 16.0, op0=ALU.mult, op1=ALU.add), reads=[B_const], writes=[B_const])
        P.dma("sp", gfin[:], gfinT[:, :], writes=[B_const])
        P.dma("sp", psc[:], pscT[:, :], writes=[B_const])
        P.dma("sp", hflag[:], haloflag.partition_broadcast(128), writes=[B_const])

        def rms_rstd(src, n, rstd, B_src, B_rstd):
            bk, bb = pb()
            for dc in range(8):
                i = rotn("sqb")
                P.op("act", lambda e, dc=dc, i=i: e.activation(sqb[i][:, 0:n], src(dc), AF.Square), reads=[B_src], writes=[B_sqb[i]])
                P.op("pe", lambda e, dc=dc, i=i: e.matmul(bk[:, 0:n], lhsT=ones_b[:], rhs=sqb[i][:, 0:n], start=(dc == 0), stop=(dc == 7)),
                     reads=[B_sqb[i], B_const], writes=[bb])
            P.op("act", lambda e: e.activation(rstd, bk[:, 0:n], AF.Sqrt, bias=epsT[:], scale=1.0 / D), reads=[bb, B_const], writes=[B_rstd])
            P.op("dve", lambda e: e.reciprocal(rstd, rstd), reads=[B_rstd], writes=[B_rstd])

        def modulate(src, n, rstd, dst, B_src, B_rstd, B_dst, shift, gs):
            for dc in range(8):
                i = rotn("sqr")
                P.op("dve", lambda e, dc=dc, i=i: e.tensor_tensor(sqr[i][:, 0:n], src(dc), rstd, ALU.mult), reads=[B_src, B_rstd], writes=[B_sqr[i]])
                P.op("act", lambda e, dc=dc, i=i: e.activation(dst(dc), sqr[i][:, 0:n], AF.Identity, bias=shift(dc), scale=gs[:, dc:dc + 1]),
                     reads=[B_sqr[i], B_mod], writes=[B_dst])

        with ExitStack() as ph:
            T = lambda name, shape, dt: ph.enter_context(nc.sbuf_tensor(name, list(shape), dt))
            cact = T("cact", [128, 8], F32); bada = T("bada", [128, 48], F32)
            gmix = T("gmix", [128, 8], F32); gffn = T("gffn", [128, 8], F32)
            tmp8 = T("tmp8", [128, 8], F32)
            wa = [T("wa%d" % i, [128, 8, 1024], F32) for i in range(3)]
            B_wa = [Buf(), Buf(), Buf()]; B_c = Buf()
            P.dma("sp", cact[:], cT[:, :], writes=[B_c])
            P.dma("sp", bada[:], badaT[:, :], writes=[B_c])
            P.dma("sp", gmix[:], gmixT[:, :], writes=[B_c])
            P.dma("sp", gffn[:], gffnT[:, :], writes=[B_c])
            P.op("act", lambda e: e.activation(cact[:], cact[:], AF.Silu), reads=[B_c], writes=[B_c])
            w_ada_v = w_ada.rearrange("(dc p) f -> p dc f", p=128)
            mbk, mbb = pb()
            for pc in range(6):
                w_, bw = wa[pc % 3], B_wa[pc % 3]
                P.dma("sp", w_[:], w_ada_v[:, :, pc * 1024:(pc + 1) * 1024], writes=[bw])
                for fl in range(8):
                    fc = pc * 8 + fl
                    for dc in range(8):
                        P.op("pe", lambda e, w_=w_, fl=fl, fc=fc, dc=dc: e.matmul(
                            mbk[:, fc:fc + 1], lhsT=w_[:, dc, fl * 128:(fl + 1) * 128], rhs=cact[:, dc:dc + 1],
                            start=(dc == 0), stop=(dc == 7)), reads=[bw, B_c], writes=[mbb])
            P.op("dve", lambda e: e.tensor_tensor(modT[:], mbk[:, 0:48], bada[:], ALU.add), reads=[mbb, B_c], writes=[B_mod])
            P.op("dve", lambda e: e.tensor_scalar(tmp8[:], modT[:, 8:16], 1.0, None, op0=ALU.add), reads=[B_mod], writes=[B_c])
            P.op("dve", lambda e: e.tensor_tensor(gs1[:], tmp8[:], gmix[:], ALU.mult), reads=[B_c], writes=[B_mod])
            P.op("dve", lambda e: e.tensor_scalar(tmp8[:], modT[:, 32:40], 1.0, None, op0=ALU.add), reads=[B_mod, B_c], writes=[B_c])
            P.op("dve", lambda e: e.tensor_tensor(gs2[:], tmp8[:], gffn[:], ALU.mult), reads=[B_c], writes=[B_mod])
            if debug:
                o = dbg_out("modT", [128, 48])
                P.dma("sp", o[:, :], modT[:], reads=[B_mod])
            P.barrier(nopool=True)
        SHIFT1 = lambda dc: modT[:, dc:dc + 1]
        GATE1 = lambda dc: modT[:, 16 + dc:17 + dc]
        SHIFT2 = lambda dc: modT[:, 24 + dc:25 + dc]
        GATE2 = lambda dc: modT[:, 40 + dc:41 + dc]
        if stop_after == 0:
            return nc, dbg

        conv = ExitStack()
        NCV = 4
        CW = 512
        cvf = [conv.enter_context(nc.sbuf_tensor("cvf%d" % i, [128, CW], F32, side="right")) for i in range(NCV)]
        cvb = [conv.enter_context(nc.sbuf_tensor("cvb%d" % i, [128, CW], BF16, side="right")) for i in range(NCV)]
        B_cvf = [Buf() for _ in range(NCV)]; B_cvb = [Buf() for _ in range(NCV)]
        B_uTs = Buf(); B_v2s = Buf()
        if stop_after is None or stop_after >= 4:
            k = 0
            for (src, dst, bd, nrow) in ((uT, uT_scr, B_uTs, 8), (vtab, v2_scr, B_v2s, 128)):
                srcv = src.rearrange("(r p) c -> r p c", p=128)
                is_u = (nrow == 8)
                ncol = src.shape[1] // CW
                jobs_cv = [(r, cc) for r in range(nrow) for cc in range(ncol)]
                for n_, (r, cc) in enumerate(jobs_cv[:NCV - 1]):
                    P.dma("pool", cvf[(k + n_) % NCV][:], srcv[r, :, cc * CW:(cc + 1) * CW], writes=[B_cvf[(k + n_) % NCV]])
                for n_, (r, cc) in enumerate(jobs_cv):
                    if n_ + NCV - 1 < len(jobs_cv):
                        r2, cc2 = jobs_cv[n_ + NCV - 1]
                        P.dma("pool", cvf[(k + NCV - 1) % NCV][:], srcv[r2, :, cc2 * CW:(cc2 + 1) * CW], writes=[B_cvf[(k + NCV - 1) % NCV]])
                    f, b_ = cvf[k % NCV], cvb[k % NCV]
                    P.op("pool", lambda e, f=f, b_=b_: e.tensor_copy(b_[:], f[:]), reads=[B_cvf[k % NCV]], writes=[B_cvb[k % NCV]])
                    dst_ap = dst[cc, :, r, :] if is_u else dst[r // 4, :, r % 4, cc * CW:(cc + 1) * CW]
                    P.dma("pool", dst_ap, b_[:], reads=[B_cvb[k % NCV]], writes=[bd])
                    k += 1

        mid = ExitStack()
        attnT = mid.enter_context(nc.sbuf_tensor("attnT", [128, 4, TOK], BF16))
        B_att = Buf()
        midq = ExitStack()
        TM = lambda name, shape, dt: midq.enter_context(nc.sbuf_tensor(name, list(shape), dt))
        Qaug = TM("Qaug", [96, 8, TOK], BF16)
        kmT = TM("kmT", [64, 8, 32], BF16)
        B_Q = [Buf() for _ in range(8)]; B_Qm = [Buf() for _ in range(8)]; B_km = Buf()
        B_kscr = Buf(); B_vscr = Buf()
        xT_v = xT.rearrange("(dc p) t -> p dc t", p=128)

        with ExitStack() as ph:
            T = lambda name, shape, dt: ph.enter_context(nc.sbuf_tensor(name, list(shape), dt))
            win = T("win", [128, 8, 1536], BF16)
            wsw = T("wsw", [128, 8, 1024], BF16)
            xc = [T("xc%d" % i, [128, 8, 512], F32) for i in range(2)]; B_xc = [Buf(), Buf()]
            B_w = Buf()
            kms = T("kms", [128, 4, 32], F32); B_kms = Buf()
            pieces = []
            w_in_v = w_in.rearrange("(dc p) f -> p dc f", p=128)
            for i in range(3):
                pieces.append((w_in_v[:, :, 512 + i * 512:512 + (i + 1) * 512], win[:, :, i * 512:(i + 1) * 512]))
            w_sw_v = w_sw.rearrange("(dc p) f -> p dc f", p=128)
            for i in range(2):
                pieces.append((w_sw_v[:, :, i * 512:(i + 1) * 512], wsw[:, :, i * 512:(i + 1) * 512]))
            for i, (src, dst) in enumerate(pieces):
                st_, bs = xc[i % 2], B_xc[i % 2]
                P.dma("sp", st_[:], src, writes=[bs])
                if i % 2 == 0:
                    P.op("dve", lambda e, dst=dst, st_=st_: e.tensor_copy(dst, st_[:]), reads=[bs], writes=[B_w])
                else:
                    P.op("act", lambda e, dst=dst, st_=st_: e.copy(dst, st_[:]), reads=[bs], writes=[B_w])
            cs = [T("cs%d" % i, [128, 2, 512], F32) for i in range(2)]; B_cs = [Buf(), Buf()]
            rstd = T("rstd", [128, 512], F32); B_rstd = Buf()
            hT = [T("hT%d" % i, [128, 8, 512], BF16) for i in range(2)]; B_hT = [Buf(), Buf()]
            t1 = [T("t1_%d" % i, [128, 512], F32) for i in range(2)]; B_t1 = [Buf(), Buf()]
            t2 = [T("t2_%d" % i, [128, 512], F32) for i in range(2)]; B_t2 = [Buf(), Buf()]
            kb = [T("kb%d" % i, [128, 512], BF16) for i in range(4)]; B_kb = [Buf() for _ in range(4)]
            vb = [T("vb%d" % i, [128, 512], BF16) for i in range(4)]; B_vb = [Buf() for _ in range(4)]

            def load_x(ci):
                P.dma("sp", xc[ci % 2][:], xT_v[:, :, ci * 512:(ci + 1) * 512], writes=[B_xc[ci % 2]])

            def load_cs(ci):
                P.dma("sp", cs[ci % 2][:, 0, :], cosF[:, ci * 512:(ci + 1) * 512], writes=[B_cs[ci % 2]])
                P.dma("sp", cs[ci % 2][:, 1, :], sinF[:, ci * 512:(ci + 1) * 512], writes=[B_cs[ci % 2]])

            def norm(ci):
                x_, bx = xc[ci % 2], B_xc[ci % 2]
                h_, bh = hT[ci % 2], B_hT[ci % 2]
                rms_rstd(lambda dc: x_[:, dc, :], 512, rstd[:], bx, B_rstd)
                modulate(lambda dc: x_[:, dc, :], 512, rstd[:], lambda dc: h_[:, dc, :], bx, B_rstd, bh, SHIFT1, gs1)

            def proj(ci):
                own = ci >= 12
                oc0 = (ci - 12) * 512
                c_, bc_ = cs[ci % 2], B_cs[ci % 2]
                h_, bh = hT[ci % 2], B_hT[ci % 2]

                def proj_fm(col0, wt, bk):
                    for dc in range(8):
                        P.op("pe", lambda e, dc=dc: e.matmul(bk[0][:, :], lhsT=wt[:, dc, col0:col0 + 128], rhs=h_[:, dc, :],
                                                             start=(dc == 0), stop=(dc == 7)), reads=[B_w, bh], writes=[bk[1]])

                def rope_pair(col_main, col_sw):
                    k1 = pb(); proj_fm(col_main, win, k1)
                    k2 = pb(); proj_fm(col_sw, wsw, k2)
                    i = rotn("t1")
                    P.op("dve", lambda e: e.tensor_tensor(t1[i][:], k1[0][:, :], c_[:, 0, :], ALU.mult), reads=[k1[1], bc_], writes=[B_t1[i]])
                    P.op("dve", lambda e: e.tensor_tensor(t2[i][:], k2[0][:, :], c_[:, 1, :], ALU.mult), reads=[k2[1], bc_], writes=[B_t2[i]])
                    P.op("dve", lambda e: e.tensor_tensor(t1[i][:], t1[i][:], t2[i][:], ALU.add), reads=[B_t1[i], B_t2[i]], writes=[B_t1[i]])
                    return i

                for m in range(4):
                    i = rope_pair(512 + m * 128, 512 + m * 128)
                    P.op("dve", lambda e, m=m, i=i: e.tensor_reduce(kms[:, m, 2 * ci:2 * ci + 2], t1[i][:].rearrange("p (b k) -> p b k", b=2),
                                                                    AX.X, ALU.add), reads=[B_t1[i]], writes=[B_kms])
                    j = rotn("kb", 4)
                    P.op("act", lambda e, i=i, j=j: e.copy(kb[j][:], t1[i][:]), reads=[B_t1[i]], writes=[B_kb[j]])
                    P.dma("sp", kT_scr[2 * m, :, ci * 512:(ci + 1) * 512], kb[j][0:64, :], reads=[B_kb[j]], writes=[B_kscr])
                    P.dma("sp", kT_scr[2 * m + 1, :, ci * 512:(ci + 1) * 512], kb[j][64:128, :], reads=[B_kb[j]], writes=[B_kscr])
                for tt in range(4):
                    bk, bb = pb()
                    for dc in range(8):
                        P.op("pe", lambda e, dc=dc, tt=tt, bk=bk: e.matmul(bk[:, :], lhsT=h_[:, dc, tt * 128:(tt + 1) * 128], rhs=win[:, dc, 1024:1536],
                                                                           start=(dc == 0), stop=(dc == 7)), reads=[B_w, bh], writes=[bb])
                    j = rotn("vb", 4)
                    P.op("act", lambda e, j=j, bk=bk: e.copy(vb[j][:], bk[:, :]), reads=[bb], writes=[B_vb[j]])
                    tok0 = ci * 512 + tt * 128
                    P.dma("sp", v_scr[tok0:tok0 + 128, :], vb[j][:], reads=[B_vb[j]], writes=[B_vscr])
                if not own:
                    return
                for m in range(4):
                    i = rope_pair(m * 128, m * 128)
                    P.op("act", lambda e, i=i, m=m: e.mul(Qaug[0:64, 2 * m, oc0:oc0 + 512], t1[i][0:64, :], 0.125), reads=[B_t1[i]], writes=[B_Q[2 * m]])
                    P.op("act", lambda e, i=i, m=m: e.mul(Qaug[0:64, 2 * m + 1, oc0:oc0 + 512], t1[i][64:128, :], 0.125), reads=[B_t1[i]], writes=[B_Q[2 * m + 1]])

            load_x(0); load_x(1); load_cs(0)
            norm(0)
            for ci in range(NCH):
                if ci + 1 < NCH:
                    load_cs(ci + 1)
                    norm(ci + 1)
                if ci + 2 < NCH:
                    load_x(ci + 2)
                proj(ci)
            for m in range(4):
                P.op("dve", lambda e, m=m: e.tensor_scalar(kmT[:, 2 * m, :], kms[0:64, m, :], 1.0 / 256, None, op0=ALU.mult), reads=[B_kms], writes=[B_km])
                P.op("act", lambda e, m=m: e.mul(kmT[:, 2 * m + 1, :], kms[64:128, m, :], 1.0 / 256), reads=[B_kms], writes=[B_km])
            if debug:
                o = dbg_out("Qaug", [96, 8, TOK], BF16)
                P.dma("sp", o, Qaug[:], reads=B_Q)
                o = dbg_out("kmT", [64, 8, 32], BF16)
                P.dma("sp", o, kmT[:], reads=[B_km])
                o = dbg_out("kT", [8, 64, S], BF16)
                P.dma("sp", o, kT_scr, reads=[B_kscr])
                o = dbg_out("v", [S, 512], BF16)
                P.dma("sp", o, v_scr, reads=[B_vscr])
            P.barrier(nopool=True)
        if stop_after == 1:
            conv.close(); midq.close(); mid.close()
            return nc, dbg

        c1w = ExitStack()
        TR = lambda name, shape, dt: c1w.enter_context(nc.sbuf_tensor(name, list(shape), dt, side="right"))
        wpg = TR("wpg", [128, 8, 2560], BF16)
        pw = TR("pw", [128, 4, 128], BF16)
        wbpb = TR("wbpb", [128, 4, 1024], BF16)
        wbab = TR("wbab", [128, 4, 1024], BF16)
        woutb = TR("woutb", [128, 8, 1024], BF16)
        B_wc1 = Buf()
        with ExitStack() as ph:
            T = lambda name, shape, dt: ph.enter_context(nc.sbuf_tensor(name, list(shape), dt))
            Kaug = [T("Kaug%d" % i, [96, S], BF16) for i in range(2)]; B_K = [Buf(), Buf()]
            Vaug = [T("Vaug%d" % i, [128, 64, 65], BF16) for i in range(2)]; B_V = [Buf(), Buf()]
            Pt = [T("Pt%d" % i, [128, 2, 512], BF16) for i in range(3)]; B_Pt = [Buf() for _ in range(3)]
            gbias = T("gbias", [128, 8, 32], F32)
            gb = [T("gb%d" % i, [128, 8, 32], F32) for i in range(2)]; B_gb = [Buf(), Buf()]
            mx8 = [T("mx8%d" % i, [128, 8, 8], F32) for i in range(2)]; B_mx = [[Buf() for _ in range(8)] for _ in range(2)]
            thr = [T("thr%d" % i, [128, 8, 1], F32) for i in range(2)]; B_thr = [Buf(), Buf()]
            stage = [T("stage%d" % i, [128, 8, 96], BF16) for i in range(2)]; B_stage = [Buf(), Buf()]
            Osb = T("Osb", [65, 512], F32); B_O = Buf()
            B_c2 = Buf()
            for i in range(2):
                P.op("dve", lambda e, i=i: e.tensor_scalar(Kaug[i][64:96, :].rearrange("p (b k) -> p b k", b=32),
                                                           iota_f[64:96, 0:32].unsqueeze(2).to_broadcast([32, 32, 256]),
                                                           pidx64[64:96, 0:1], None, op0=ALU.is_equal), reads=[B_const], writes=[B_K[i]])
            P.op("dve", lambda e: e.memset(gbias[:], -1e30), writes=[B_c2])
            for s_ in range(8):
                P.dma("sp", gbias[:, s_, 0:24], pastb.partition_broadcast(128), reads=[B_c2], writes=[B_c2])
            for s_ in range(1, 8):
                P.op("dve", lambda e, s_=s_: e.memset(gbias[:, s_, 24:24 + s_], 0.0), reads=[B_c2], writes=[B_c2])
            for i in range(2):
                P.op("dve", lambda e, i=i: e.memset(stage[i][:], 0.0), writes=[B_stage[i]])
                P.op("dve", lambda e, i=i: e.memset(Vaug[i][:, :, 64:65], 1.0), writes=[B_V[i]])

            def load_head(h):
                i = h % 2
                P.dma("sp", Kaug[i][0:64, :], kT_scr[h, :, :], reads=[B_kscr], writes=[B_K[i]])
                P.dma("sp", Vaug[i][:, :, 0:64], v_scr[:, h * 64:(h + 1) * 64].rearrange("(kt p) c -> p kt c", p=128), reads=[B_vscr], writes=[B_V[i]])

            load_head(0)
            load_head(1)
            pieces = []
            w_in_v = w_in.rearrange("(dc p) f -> p dc f", p=128)
            for i in range(8):
                pieces.append((w_in_v[:, :, i * 64:(i + 1) * 64], wpg[:, :, i * 64:(i + 1) * 64], (8, 64)))
            for i in range(32):
                pieces.append((w_in_v[:, :, 2048 + i * 64:2048 + (i + 1) * 64], wpg[:, :, 512 + i * 64:512 + (i + 1) * 64], (8, 64)))
            pieces.append((pool_w.rearrange("g i o -> i g o"), pw[:, :, :], (4, 128)))
            wbp_v = wbp.rearrange("(g p) f -> p g f", p=128)
            wba_v = wba.rearrange("(g p) f -> p g f", p=128)
            for i in range(8):
                pieces.append((wbp_v[:, :, i * 128:(i + 1) * 128], wbpb[:, :, i * 128:(i + 1) * 128], (4, 128)))
            for i in range(8):
                pieces.append((wba_v[:, :, i * 128:(i + 1) * 128], wbab[:, :, i * 128:(i + 1) * 128], (4, 128)))
            wo_v = w_out.rearrange("(dc p) f -> p dc f", p=128)
            for i in range(16):
                pieces.append((wo_v[:, :, i * 64:(i + 1) * 64], woutb[:, :, i * 64:(i + 1) * 64], (8, 64)))
            stg_state = {"dma": 0, "cast": 0}

            def stg_view(k):
                a, b_ = pieces[k][2]
                return sqr[k % 2][:, 0:a * b_].rearrange("p (a b) -> p a b", a=a)

            def stage_step(ndma):
                while stg_state["cast"] < stg_state["dma"]:
                    k = stg_state["cast"]
                    P.op("dve", lambda e, k=k: e.tensor_copy(pieces[k][1], stg_view(k)), reads=[B_sqr[k % 2]], writes=[B_wc1])
                    stg_state["cast"] += 1
                for _ in range(ndma):
                    k = stg_state["dma"]
                    if k >= len(pieces):
                        break
                    P.dma("sp", stg_view(k), pieces[k][0], writes=[B_sqr[k % 2]])
                    stg_state["dma"] += 1
            for tt in range(16):
                if tt % 3 == 0:
                    stage_step(2)
                s_ = tt // 2
                r = tt % 2
                bk, bb = pb()
                for h in range(8):
                    P.op("pe", lambda e, tt=tt, bk=bk, h=h: e.matmul(bk[:, h * 32:(h + 1) * 32], lhsT=Qaug[0:64, h, tt * 128:(tt + 1) * 128], rhs=kmT[:, h, :],
                                                                     start=True, stop=True), reads=[B_Q[h], B_km], writes=[bb])
                P.op("dve", lambda e, s_=s_, bk=bk, r=r: e.tensor_tensor(gb[r][:], bk[:, 0:256].rearrange("p (h n) -> p h n", h=8),
                                                                         gbias[:, s_, :].unsqueeze(1).to_broadcast([128, 8, 32]), ALU.add),
                     reads=[bb, B_c2], writes=[B_gb[r]])
                for h in range(8):
                    P.op("dve", lambda e, r=r, h=h: e.max(out=mx8[r][:, h, :], in_=gb[r][:, h, :]), reads=[B_gb[r]], writes=[B_mx[r][h]])
                P.op("dve", lambda e, r=r: e.tensor_scalar(thr[r][:], mx8[r][:, :, 2:3], -1e29, None, op0=ALU.max), reads=B_mx[r], writes=[B_thr[r]])
                P.op("dve", lambda e, r=r: e.tensor_tensor(stage[r][:, :, 64:96], gb[r][:], thr[r][:].to_broadcast([128, 8, 32]), ALU.is_lt),
                     reads=[B_gb[r], B_thr[r]], writes=[B_stage[r]])
                P.op("dve", lambda e, r=r, s_=s_: e.memset(stage[r][:, :, 64 + 24 + s_:64 + 25 + s_], 0.0), reads=[B_stage[r]], writes=[B_stage[r]])
                for h in range(8):
                    bk2, bb2 = pb()
                    trv = bk2[:, :].bitcast(BF16)
                    P.op("pe", lambda e, trv=trv, r=r, h=h: e.transpose(trv[0:96, 0:128], stage[r][:, h, :], ident_b[:]), reads=[B_stage[r], B_const], writes=[bb2])
                    P.op("act", lambda e, tt=tt, trv=trv, h=h: e.mul(Qaug[64:96, h, tt * 128:(tt + 1) * 128], trv[64:96, 0:128], NEGM), reads=[bb2], writes=[B_Qm[h]])
            LOOK = 2
            for h in range(8):
                if 1 <= h < 7:
                    load_head(h + 1)
                Kh, bK = Kaug[h % 2], B_K[h % 2]
                Vh, bV = Vaug[h % 2], B_V[h % 2]
                for c in range(4):
                    stage_step(2)
                    oacc, ob = banks[7], bbuf[7]
                    rot["set"] = [6]
                    jobs = [(kt, 0, 512, None) for kt in range(0, 48, 2)]
                    for m in range(2 * c + 2):
                        kt = 48 + 2 * m
                        if m < 2 * c:
                            jobs.append((kt, 0, 512, None))
                        elif m == 2 * c:
                            jobs.append((kt, 0, 512, 0))
                        else:
                            jobs.append((kt, 256, 512, 256))
                    q0 = c * 512
                    nj = len(jobs)
                    pts = [None] * nj
                    for ji in range(nj + LOOK):
                        if ji < nj:
                            kt, c0, c1, dg = jobs[ji]
                            pp = rotn("spair", 3)
                            pr_, pbb = pairs[pp], [bbuf[2 * pp], bbuf[2 * pp + 1]]
                            prv = pr_[:, :].rearrange("p (b n) -> p b n", b=2)
                            for hf in range(2):
                                P.op("pe", lambda e, kt=kt, c0=c0, c1=c1, prv=prv, hf=hf: e.matmul(prv[:, hf, c0:c1], lhsT=Kh[:, (kt + hf) * 128:(kt + hf + 1) * 128],
                                                                                                   rhs=Qaug[:, h, q0 + c0:q0 + c1], start=True, stop=True),
                                     reads=[bK, B_Q[h], B_Qm[h]], writes=[pbb[hf]])
                            pi = rotn("pt", 3)
                            p_, bp = Pt[pi], B_Pt[pi]
                            pts[ji] = (p_, bp)
                            P.op("act", lambda e, c0=c0, c1=c1, prv=prv, p_=p_: e.activation(p_[:, :, c0:c1], prv[:, :, c0:c1], AF.Exp), reads=pbb, writes=[bp])
                            if dg is not None:
                                P.op("dve", lambda e, dg=dg, p_=p_: e.tensor_tensor(p_[:, :, dg:dg + 256], p_[:, :, dg:dg + 256], tri[:, :, :], ALU.mult),
                                     reads=[bp, B_const], writes=[bp])
                        jv = ji - LOOK
                        if jv >= 0:
                            kt, c0, c1, dg = jobs[jv]
                            p_, bp = pts[jv]
                            for hf in range(2):
                                P.op("pe", lambda e, kt=kt, c0=c0, c1=c1, p_=p_, jv=jv, hf=hf: e.matmul(oacc[0:65, c0:c1], lhsT=Vh[:, kt + hf, :], rhs=p_[:, hf, c0:c1],
                                                                                                      start=(jv == 0 and hf == 0), stop=(jv == nj - 1 and hf == 1)),
                                     reads=[bV, bp], writes=[ob])
                    P.op("act", lambda e: e.copy(Osb[:], oacc[0:65, :]), reads=[ob], writes=[B_O])
                    P.op("dve", lambda e: e.reciprocal(Osb[64:65, :], Osb[64:65, :]), reads=[B_O], writes=[B_O])
                    bk, bb = pb()
                    P.op("pe", lambda e, bk=bk: e.matmul(bk[0:64, :], lhsT=ones_f[64:65, 0:64], rhs=Osb[64:65, :], start=True, stop=True),
                         reads=[B_O, B_const], writes=[bb])
                    P.op("dve", lambda e, bk=bk: e.tensor_tensor(Osb[0:64, :], Osb[0:64, :], bk[0:64, :], ALU.mult), reads=[B_O, bb], writes=[B_O])
                    pr = 64 * (h % 2)
                    P.op("act", lambda e, c=c, pr=pr: e.copy(attnT[pr:pr + 64, h // 2, c * 512:(c + 1) * 512], Osb[0:64, :]), reads=[B_O], writes=[B_att])
                    rot["set"] = list(range(8))
            while stg_state["cast"] < len(pieces):
                stage_step(2)
            if debug:
                o = dbg_out("attnT", [128, 4, TOK], BF16)
                P.dma("sp", o, attnT[:], reads=[B_att])
            P.barrier(nopool=True)
        midq.close()
        if stop_after == 2:
            mid.close(); c1w.close(); conv.close()
            return nc, dbg

        B_x1s = Buf()
        x1s_v = x1_scr.rearrange("(dc p) t -> p dc t", p=128)
        with ExitStack() as ph:
            T = lambda name, shape, dt: ph.enter_context(nc.sbuf_tensor(name, list(shape), dt))
            NT = 256
            icn = T("icn", [128, 64], F32)
            B_w = B_wc1
            P.dma("sp", icn[:], invcnt.partition_broadcast(128), writes=[B_w])
            xc2 = [T("xcc%d" % i, [128, 8, NT], F32) for i in range(2)]; B_xc2 = [Buf(), Buf()]
            rstd = T("rstdc", [128, NT], F32); B_rstd = Buf()
            h2_ = [T("hTc%d" % i, [128, 8, NT], BF16) for i in range(2)]; B_h2_ = [Buf(), Buf()]
            ub = T("ub", [128, 4, 16 + NT], F32); B_ub = Buf()
            sw_ = T("sw_", [128, 16 + NT], F32); sx_ = T("sx_", [128, 16 + NT], F32); B_s = Buf()
            dif = T("dif", [128, 4, NT], BF16); B_dif = Buf()
            mixT = T("mixT", [128, 4, NT], BF16); B_mix = Buf()
            sgp = T("sgp", [128, 8, NT], F32); B_sgp = [Buf() for _ in range(8)]
            sga = T("sga", [128, 8, NT], F32); B_sga = [Buf() for _ in range(8)]
            sg = [T("sg%d" % i, [128, NT], F32) for i in range(4)]; B_sg = [Buf() for _ in range(4)]
            mrg = T("mrg", [128, 8, NT], BF16); B_mrg = Buf()
            x1c = T("x1c", [128, 8, NT], F32); B_x1 = Buf()
            xh = T("xh", [128, 8, 16], F32); hh = T("hh", [128, 8, 16], BF16); B_xh = Buf()
            rsh = T("rsh", [128, 16], F32); B_rsh = Buf()
            W = 16 + NT

            P.dma("sp", xh[:], xhT.rearrange("(dc p) t -> p dc t", p=128), writes=[B_xh])
            rms_rstd(lambda dc: xh[:, dc, :], 16, rsh[:], B_xh, B_rsh)
            modulate(lambda dc: xh[:, dc, :], 16, rsh[:], lambda dc: hh[:, dc, :], B_xh, B_rsh, B_xh, SHIFT1, gs1)
            for g in range(4):
                bk, bb = pb()
                for dc in range(8):
                    P.op("pe", lambda e, g=g, dc=dc, bk=bk: e.matmul(bk[:, 0:16], lhsT=wpg[:, dc, g * 128:(g + 1) * 128], rhs=hh[:, dc, :],
                                                                     start=(dc == 0), stop=(dc == 7)), reads=[B_w, B_xh], writes=[bb])
                P.op("dve", lambda e, g=g, bk=bk: e.tensor_scalar(ub[:, g, NT:W], bk[:, 0:16], hflag[:, 0:1], None, op0=ALU.mult),
                     reads=[bb, B_const], writes=[B_ub])

            def norm1(ci):
                c0 = ci * NT
                xc, B_xc = xc2[ci % 2], B_xc2[ci % 2]
                h_, bh = h2_[ci % 2], B_h2_[ci % 2]
                P.dma("sp", xc[:], xT_v[:, :, 6144 + c0:6144 + c0 + NT], writes=[B_xc])
                rms_rstd(lambda dc: xc[:, dc, :], NT, rstd[:], B_xc, B_rstd)
                modulate(lambda dc: xc[:, dc, :], NT, rstd[:], lambda dc: h_[:, dc, :], B_xc, B_rstd, bh, SHIFT1, gs1)

            def rest(ci):
                c0 = ci * NT
                xc, B_xc = xc2[ci % 2], B_xc2[ci % 2]
                h_, bh = h2_[ci % 2], B_h2_[ci % 2]

                def proj_fm(col0, bk):
                    for dc in range(8):
                        P.op("pe", lambda e, dc=dc: e.matmul(bk[0][:, 0:NT], lhsT=wpg[:, dc, col0:col0 + 128], rhs=h_[:, dc, :],
                                                             start=(dc == 0), stop=(dc == 7)), reads=[B_w, bh], writes=[bk[1]])

                P.op("dve", lambda e: e.tensor_copy(ub[:, :, 0:16], ub[:, :, NT:W]), reads=[B_ub], writes=[B_ub])
                for g in range(4):
                    bk = pb(); proj_fm(g * 128, bk)
                    P.op("act", lambda e, g=g, bk=bk: e.copy(ub[:, g, 16:W], bk[0][:, 0:NT]), reads=[bk[1], B_ub], writes=[B_ub])
                for fc in range(8):
                    kg = pb(); proj_fm(512 + fc * 128, kg)
                    P.op("act", lambda e, fc=fc, kg=kg: e.activation(sgp[:, fc, :], kg[0][:, 0:NT], AF.Sigmoid), reads=[kg[1]], writes=[B_sgp[fc]])
                    ka = pb(); proj_fm(1536 + fc * 128, ka)
                    P.op("act", lambda e, fc=fc, ka=ka: e.activation(sga[:, fc, :], ka[0][:, 0:NT], AF.Sigmoid), reads=[ka[1]], writes=[B_sga[fc]])
                for g in range(4):
                    wdw = 2 << g
                    cur = ub[:, g, :]
                    sh = 1
                    for step in range(g + 1):
                        dst = sw_ if step % 2 == 0 else sx_
                        lo = 2 * sh - 1
                        P.op("dve", lambda e, cur=cur, dst=dst, sh=sh, lo=lo: e.tensor_tensor(
                            dst[:, lo:W], cur[:, lo:W], cur[:, lo - sh:W - sh], ALU.add), reads=[B_ub, B_s], writes=[B_s])
                        cur = dst[:, :]
                        sh *= 2
                    P.op("dve", lambda e, g=g, cur=cur, wdw=wdw: e.scalar_tensor_tensor(
                        dif[:, g, :], cur[:, 16:W], 1.0 / wdw, ub[:, g, 16:W], op0=ALU.mult, op1=ALU.subtract),
                        reads=[B_s, B_ub, B_dif], writes=[B_dif])
                    if ci == 0:
                        P.op("dve", lambda e, g=g, cur=cur: e.tensor_tensor(rsh[:], cur[:, 16:32], icn[:, g * 16:(g + 1) * 16], ALU.mult),
                             reads=[B_s, B_w, B_rsh], writes=[B_rsh])
                        P.op("dve", lambda e, g=g: e.tensor_tensor(dif[:, g, 0:16], rsh[:], ub[:, g, 16:32], ALU.subtract),
                             reads=[B_rsh, B_ub, B_dif], writes=[B_dif])
                for g in range(4):
                    bk, bb = pb()
                    P.op("pe", lambda e, g=g, bk=bk: e.matmul(bk[:, 0:NT], lhsT=pw[:, g, :], rhs=dif[:, g, :], start=True, stop=True),
                         reads=[B_w, B_dif], writes=[bb])
                    P.op("dve", lambda e, g=g, bk=bk: e.tensor_scalar(mixT[:, g, :], bk[:, 0:NT], psc[:, g:g + 1], None, op0=ALU.mult),
                         reads=[bb, B_const, B_mix], writes=[B_mix])
                for fc in range(8):
                    kp, kpb = pb()
                    for g in range(4):
                        P.op("pe", lambda e, g=g, fc=fc, kp=kp: e.matmul(kp[:, 0:NT], lhsT=wbpb[:, g, fc * 128:(fc + 1) * 128], rhs=mixT[:, g, :],
                                                                         start=(g == 0), stop=(g == 3)), reads=[B_w, B_mix], writes=[kpb])
                    kq, kqb = pb()
                    for hp in range(4):
                        P.op("pe", lambda e, hp=hp, fc=fc, kq=kq: e.matmul(kq[:, 0:NT], lhsT=wbab[:, hp, fc * 128:(fc + 1) * 128], rhs=attnT[:, hp, c0:c0 + NT],
                                                                           start=(hp == 0), stop=(hp == 3)), reads=[B_w, B_att], writes=[kqb])
                    j = rotn("sg", 4)
                    j2 = rotn("sg", 4)
                    P.op("dve", lambda e, j=j, fc=fc, kp=kp: e.tensor_tensor(sg[j][:], kp[:, 0:NT], sgp[:, fc, :], ALU.mult),
                         reads=[kpb, B_sgp[fc]], writes=[B_sg[j]])
                    P.op("dve", lambda e, j2=j2, fc=fc, kq=kq: e.tensor_tensor(sg[j2][:], kq[:, 0:NT], sga[:, fc, :], ALU.mult),
                         reads=[kqb, B_sga[fc]], writes=[B_sg[j2]])
                    P.op("dve", lambda e, j=j, j2=j2, fc=fc: e.tensor_tensor(mrg[:, fc, :], sg[j2][:], sg[j][:], ALU.add),
                         reads=[B_sg[j], B_sg[j2], B_mrg], writes=[B_mrg])
                for oc in range(8):
                    bk, bb = pb()
                    for fc in range(8):
                        P.op("pe", lambda e, oc=oc, fc=fc, bk=bk: e.matmul(bk[:, 0:NT], lhsT=woutb[:, fc, oc * 128:(oc + 1) * 128], rhs=mrg[:, fc, :],
                                                                           start=(fc == 0), stop=(fc == 7)), reads=[B_w, B_mrg], writes=[bb])
                    P.op("dve", lambda e, oc=oc, bk=bk: e.scalar_tensor_tensor(x1c[:, oc, :], bk[:, 0:NT], GATE1(oc), xc[:, oc, :], op0=ALU.mult, op1=ALU.add),
                         reads=[bb, B_mod, B_xc, B_x1], writes=[B_x1])
                P.dma("sp", x1s_v[:, :, c0:c0 + NT], x1c[:], reads=[B_x1], writes=[B_x1s])

            norm1(0)
            for ci in range(TOK // NT):
                if ci + 1 < TOK // NT:
                    norm1(ci + 1)
                rest(ci)
            if debug:
                o = dbg_out("x1T", [D, TOK])
                P.dma("sp", o, x1_scr, reads=[B_x1s])
            P.barrier(nopool=True)
        mid.close()
        c1w.close()
        if stop_after == 3:
            conv.close()
            return nc, dbg

        late = ExitStack()
        TL = lambda name, shape, dt: late.enter_context(nc.sbuf_tensor(name, list(shape), dt))
        h2T = TL("h2T", [128, 8, TOK], BF16); B_h2 = Buf()
        iT = TL("iT", [128, TOK], F32); jT = TL("jT", [128, TOK], F32); gT = TL("gT", [128, TOK], F32)
        B_ijg = Buf()
        with ExitStack() as ph:
            T = lambda name, shape, dt: ph.enter_context(nc.sbuf_tensor(name, list(shape), dt))
            wqb = T("wqb", [128, 8, 2048], BF16)
            skb = T("skb", [128, 16, 128], BF16)
            B_w = Buf()
            stg_scope = ExitStack()
            x1c = stg_scope.enter_context(nc.sbuf_tensor("stg1", [128, 8, 512], F32)); B_x1 = Buf()
            stg2 = stg_scope.enter_context(nc.sbuf_tensor("stg2", [128, 8, 512], F32)); B_stg2 = Buf()
            pieces = []
            wq_v = wq.rearrange("(dc p) f -> p dc f", p=128)
            for i in range(4):
                pieces.append((wq_v[:, :, i * 512:(i + 1) * 512], wqb[:, :, i * 512:(i + 1) * 512], (8, 512)))
            for i in range(4):
                pieces.append((skT[:, i * 4:(i + 1) * 4, :], skb[:, i * 4:(i + 1) * 4, :], (4, 128)))
            stgs = [(x1c, B_x1), (stg2, B_stg2)]
            for i, (src, dst, (a, b_)) in enumerate(pieces):
                st_, bs = stgs[i % 2]
                sv = st_[:, 0:a, 0:b_]
                P.dma("sp", sv, src, writes=[bs])
                if i % 2 == 0:
                    P.op("dve", lambda e, dst=dst, sv=sv: e.tensor_copy(dst, sv), reads=[bs], writes=[B_w])
                else:
                    P.op("act", lambda e, dst=dst, sv=sv: e.copy(dst, sv), reads=[bs], writes=[B_w])
            P.barrier(nopool=True)
            stg_scope.close()
            NQ = 256
            x1c2 = [T("x1e%d" % i, [128, 8, NQ], F32) for i in range(2)]; B_x12 = [Buf(), Buf()]
            rstd = T("rstde", [128, NQ], F32); B_rstd = Buf()
            qT2 = [T("qT%d" % i, [128, 16, NQ], BF16) for i in range(2)]; B_qT2 = [Buf(), Buf()]
            ssb = [T("ssb%d" % i, [128, 16, 128], F32) for i in range(2)]; B_ssb = [[Buf() for _ in range(16)] for _ in range(2)]
            wk16 = T("wk16", [128, 16, 128], F32); B_wk16 = [Buf() for _ in range(16)]
            wk8 = wk16[:].rearrange("p (h two) k -> p h (two k)", two=2)
            sv_ = T("sv_", [128, 16, 16], F32); sidx = T("sidx", [128, 16, 16], U32); sidf = T("sidf", [128, 16, 16], F32)
            B_sva = [Buf() for _ in range(16)]; B_svb = [Buf() for _ in range(16)]
            B_ixa = [Buf() for _ in range(16)]; B_ixb = [Buf() for _ in range(16)]; B_sidf = Buf()
            cand = T("cand", [128, 8, 256], F32); B_cand = [Buf() for _ in range(8)]
            cv = T("cv", [128, 8, 16], F32); cpos = T("cpos", [128, 8, 16], U32); cpf = T("cpf", [128, 8, 16], F32)
            B_cva = [Buf() for _ in range(8)]; B_cvb = [Buf() for _ in range(8)]
            B_cpa = [Buf() for _ in range(8)]; B_cpb = [Buf() for _ in range(8)]; B_cpf = Buf()
            ak = T("ak", [128, 8, 16], F32); bk_ = T("bk_", [128, 8, 16], F32); B_ab = Buf()
            big = T("big", [128, 8, 16, 16], F32); B_big = Buf()
            bigi = T("bigi", [128, 8, 16, 16], F32); B_bigi = Buf()
            bigj = big; B_bigj = B_big
            iK = T("iK", [128, 8, 16], F32); jK = T("jK", [128, 8, 16], F32); gK = T("gK", [128, 8, 16], F32)
            B_iK = Buf(); B_jK = Buf(); B_gK = Buf()
            ssum = T("ssum", [128, 8], F32); B_ss = Buf()
            def front(ci):
                c0 = ci * NQ
                x1c, B_x1 = x1c2[ci % 2], B_x12[ci % 2]
                qT, B_qT = qT2[ci % 2], B_qT2[ci % 2]
                P.dma("sp", x1c[:], x1s_v[:, :, c0:c0 + NQ], reads=[B_x1s], writes=[B_x1])
                rms_rstd(lambda dc: x1c[:, dc, :], NQ, rstd[:], B_x1, B_rstd)
                modulate(lambda dc: x1c[:, dc, :], NQ, rstd[:], lambda dc: h2T[:, dc, c0:c0 + NQ], B_x1, B_rstd, B_h2, SHIFT2, gs2)
                for hp in range(16):
                    bk, bb = pb()
                    for dc in range(8):
                        P.op("pe", lambda e, hp=hp, dc=dc, bk=bk: e.matmul(bk[:, 0:NQ], lhsT=wqb[:, dc, hp * 128:(hp + 1) * 128], rhs=h2T[:, dc, c0:c0 + NQ],
                                                                           start=(dc == 0), stop=(dc == 7)), reads=[B_w, B_h2], writes=[bb])
                    P.op("act", lambda e, hp=hp, bk=bk: e.copy(qT[:, hp, :], bk[:, 0:NQ]), reads=[bb, B_qT], writes=[B_qT])

            front(0)
            for ci in range(TOK // NQ):
                c0 = ci * NQ
                if ci + 1 < TOK // NQ:
                    front(ci + 1)
                qT, B_qT = qT2[ci % 2], B_qT2[ci % 2]
                for tt in range(NQ // 128):
                    t0 = c0 + tt * 128
                    rs = rotn("ssb")
                    ss_, bss = ssb[rs], B_ssb[rs]
                    for g4 in range(4):
                        bk, bb = pb()
                        for q in range(4):
                            hp = g4 * 4 + q
                            P.op("pe", lambda e, hp=hp, q=q, bk=bk: e.matmul(bk[:, q * 128:(q + 1) * 128], lhsT=qT[:, hp, tt * 128:(tt + 1) * 128], rhs=skb[:, hp, :],
                                                                             start=True, stop=True), reads=[B_qT, B_w], writes=[bb])
                        P.op("act", lambda e, g4=g4, bk=bk, ss_=ss_: e.copy(ss_[:, g4 * 4:(g4 + 1) * 4, :], bk[:, :].rearrange("p (a b) -> p a b", a=4)),
                             reads=[bb], writes=bss[g4 * 4:(g4 + 1) * 4])
                    R16 = range(16)
                    for hp in R16:
                        P.op("dve", lambda e, hp=hp: e.max(out=sv_[:, hp, 0:8], in_=ss_[:, hp, :]), reads=[bss[hp]], writes=[B_sva[hp]])
                    for hp in R16:
                        P.op("dve", lambda e, hp=hp: e.max_index(out=sidx[:, hp, 0:8], in_max=sv_[:, hp, 0:8], in_values=ss_[:, hp, :]), reads=[bss[hp], B_sva[hp]], writes=[B_ixa[hp]])
                    for hp in R16:
                        P.op("dve", lambda e, hp=hp: e.match_replace(out=wk16[:, hp, :], in_to_replace=sv_[:, hp, 0:8], in_values=ss_[:, hp, :], imm_value=-1e30),
                             reads=[bss[hp], B_sva[hp]], writes=[B_wk16[hp]])
                    for hp in R16:
                        P.op("dve", lambda e, hp=hp: e.max(out=sv_[:, hp, 8:16], in_=wk16[:, hp, :]), reads=[B_wk16[hp]], writes=[B_svb[hp]])
                    for hp in R16:
                        P.op("dve", lambda e, hp=hp: e.max_index(out=sidx[:, hp, 8:16], in_max=sv_[:, hp, 8:16], in_values=wk16[:, hp, :]), reads=[B_wk16[hp], B_svb[hp]], writes=[B_ixb[hp]])
                    P.op("dve", lambda e: e.tensor_copy(sidf[:], sidx[:]), reads=B_ixa + B_ixb, writes=[B_sidf])
                    svv = sv_[:].rearrange("p (h two) k -> p h two k", two=2)
                    sfv = sidf[:].rearrange("p (h two) k -> p h two k", two=2)
                    candv = cand[:].rearrange("p h (a b) -> p h a b", a=16)
                    P.op("dve", lambda e: e.tensor_tensor(candv, svv[:, :, 0, :].unsqueeze(3).to_broadcast([128, 8, 16, 16]),
                                                          svv[:, :, 1, :].unsqueeze(2).to_broadcast([128, 8, 16, 16]), ALU.add), reads=B_sva + B_svb, writes=B_cand)
                    R8 = range(8)
                    for h in R8:
                        P.op("dve", lambda e, h=h: e.max(out=cv[:, h, 0:8], in_=cand[:, h, :]), reads=[B_cand[h]], writes=[B_cva[h]])
                    for h in R8:
                        P.op("dve", lambda e, h=h: e.max_index(out=cpos[:, h, 0:8], in_max=cv[:, h, 0:8], in_values=cand[:, h, :]), reads=[B_cand[h], B_cva[h]], writes=[B_cpa[h]])
                    for h in R8:
                        P.op("dve", lambda e, h=h: e.match_replace(out=wk8[:, h, :], in_to_replace=cv[:, h, 0:8], in_values=cand[:, h, :], imm_value=-1e30),
                             reads=[B_cand[h], B_cva[h]], writes=[B_wk16[2 * h], B_wk16[2 * h + 1]])
                    for h in R8:
                        P.op("dve", lambda e, h=h: e.max(out=cv[:, h, 8:16], in_=wk8[:, h, :]), reads=[B_wk16[2 * h], B_wk16[2 * h + 1]], writes=[B_cvb[h]])
                    for h in R8:
                        P.op("dve", lambda e, h=h: e.max_index(out=cpos[:, h, 8:16], in_max=cv[:, h, 8:16], in_values=wk8[:, h, :]), reads=[B_wk16[2 * h], B_wk16[2 * h + 1], B_cvb[h]], writes=[B_cpb[h]])
                    P.op("dve", lambda e: e.tensor_copy(cpf[:], cpos[:]), reads=B_cpa + B_cpb, writes=[B_cpf])
                    P.op("dve", lambda e: e.tensor_tensor(gK[:], cv[:], cv[:, :, 0:1].to_broadcast([128, 8, 16]), ALU.subtract), reads=B_cva + B_cvb + [B_gK], writes=[B_gK])
                    P.op("act", lambda e: e.activation(gK[:], gK[:], AF.Exp), reads=[B_gK], writes=[B_gK])
                    th4 = thr16[:].unsqueeze(1).unsqueeze(1).to_broadcast([128, 8, 16, 16])
                    io4 = iota_f[:, 0:16].unsqueeze(1).unsqueeze(1).to_broadcast([128, 8, 16, 16])
                    P.op("dve", lambda e: e.tensor_tensor(big[:], cpf[:].unsqueeze(3).to_broadcast([128, 8, 16, 16]), th4, ALU.is_ge),
                         reads=[B_cpf, B_const, B_big], writes=[B_big])
                    P.op("dve", lambda e: e.tensor_reduce(ak[:], big[:], AX.X, ALU.add), reads=[B_big, B_ab], writes=[B_ab])
                    P.op("dve", lambda e: e.tensor_tensor(bigi[:], io4, ak[:].unsqueeze(3).to_broadcast([128, 8, 16, 16]), ALU.is_equal),
                         reads=[B_ab, B_const, B_bigi], writes=[B_bigi])
                    P.op("dve", lambda e: e.tensor_tensor(bigi[:], bigi[:], sfv[:, :, 0, :].unsqueeze(2).to_broadcast([128, 8, 16, 16]), ALU.mult),
                         reads=[B_bigi, B_sidf], writes=[B_bigi])
                    P.op("dve", lambda e: e.scalar_tensor_tensor(bk_[:], ak[:], -16.0, cpf[:], op0=ALU.mult, op1=ALU.add), reads=[B_cpf, B_ab], writes=[B_ab])
                    P.op("dve", lambda e: e.tensor_tensor(bigj[:], io4, bk_[:].unsqueeze(3).to_broadcast([128, 8, 16, 16]), ALU.is_equal),
                         reads=[B_ab, B_const, B_bigj], writes=[B_bigj])
                    P.op("dve", lambda e: e.tensor_tensor(bigj[:], bigj[:], sfv[:, :, 1, :].unsqueeze(2).to_broadcast([128, 8, 16, 16]), ALU.mult),
                         reads=[B_bigj, B_sidf], writes=[B_bigj])
                    P.op("dve", lambda e: e.tensor_reduce(ssum[:], gK[:], AX.X, ALU.add), reads=[B_gK, B_ss], writes=[B_ss])
                    P.op("dve", lambda e: e.reciprocal(ssum[:], ssum[:]), reads=[B_ss], writes=[B_ss])
                    P.op("dve", lambda e: e.tensor_tensor(gK[:], gK[:], ssum[:].unsqueeze(2).to_broadcast([128, 8, 16]), ALU.mult), reads=[B_gK, B_ss], writes=[B_gK])
                    P.op("dve", lambda e: e.tensor_reduce(iK[:], bigi[:], AX.X, ALU.add), reads=[B_bigi, B_iK], writes=[B_iK])
                    P.op("dve", lambda e: e.tensor_reduce(jK[:], bigj[:], AX.X, ALU.add), reads=[B_bigj, B_jK], writes=[B_jK])
                    for (src, dst, bsrc) in ((gK, gT, B_gK), (iK, iT, B_iK), (jK, jT, B_jK)):
                        bk, bb = pb()
                        P.op("pe", lambda e, src=src, bk=bk: e.transpose(bk[:, 0:128], src[:].rearrange("p h k -> p (h k)"), ident_f[:]), reads=[bsrc, B_const], writes=[bb])
                        P.op("act", lambda e, dst=dst, bk=bk: e.copy(dst[:, t0:t0 + 128], bk[:, 0:128]), reads=[bb], writes=[B_ijg])
            if debug:
                for name, t_ in (("iT", iT), ("jT", jT), ("gT", gT)):
                    o = dbg_out(name, [128, TOK])
                    P.dma("sp", o, t_[:], reads=[B_ijg])
                o = dbg_out("h2T", [128, 8, TOK], BF16)
                P.dma("sp", o, h2T[:], reads=[B_h2])
            P.barrier()
        conv.close()
        if stop_after == 4:
            late.close()
            return nc, dbg

        with ExitStack() as ph:
            T = lambda name, shape, dt: ph.enter_context(nc.sbuf_tensor(name, list(shape), dt))
            TC = 256
            GI = 4
            TB = 16
            NBUF = 3
            Wt = [T("Wt%d" % i, [128, TC, 64], BF16) for i in range(2)]; B_Wt = [Buf(), Buf()]
            ohi = [T("ohi%d" % i, [128, TB, 64], BF16) for i in range(2)]; B_ohi = [Buf(), Buf()]
            ohj = [T("ohj%d" % i, [128, TB, 128], BF16) for i in range(2)]; B_ohj = [Buf(), Buf()]
            ub_ = [T("ub_%d" % i, [128, 8, GI * 128], BF16) for i in range(NBUF)]; B_u = [Buf() for _ in range(NBUF)]
            vb_ = [T("vb_%d" % i, [128, GI, 1024], BF16) for i in range(NBUF)]; B_v = [Buf() for _ in range(NBUF)]
            Gt = [T("Gt%d" % i, [128, TC], BF16) for i in range(2)]; B_G = [Buf(), Buf()]
            WA = [T("WA%d" % i, [128, TC], BF16) for i in range(2)]; B_WA = [Buf(), Buf()]
            x1c = T("x1d", [128, 8, TC], F32); B_x1 = Buf()
            rstd = T("rstdd", [128, TC], F32); B_rstd = Buf()
            outT_v = outT.rearrange("(dc p) t -> p dc t", p=128)
            NG = 128 // GI
            NCHK = TOK // TC
            gcount = {"n": 0}

            def load_tables(gidx):
                ig = gidx % NG
                b_ = gidx % NBUF
                P.dma("sp", ub_[b_][:], uT_scr[ig], reads=[B_uTs], writes=[B_u[b_]])
                P.dma("sp", vb_[b_][:], v2_scr[ig], reads=[B_v2s], writes=[B_v[b_]])

            def wbuild_steps(ch, half, buf):
                tbase = ch * TC
                io_i = iota_b[:, half * 64:(half + 1) * 64].unsqueeze(1).to_broadcast([128, TB, 64])
                io_j = iota_b[:].unsqueeze(1).to_broadcast([128, TB, 128])

                def onehots(tb):
                    tg = tbase + tb * TB
                    r = tb % 2
                    P.op("dve", lambda e: e.tensor_tensor(ohj[r][:], io_j, jT[:, tg:tg + TB].unsqueeze(2).to_broadcast([128, TB, 128]), ALU.is_equal),
                         reads=[B_const, B_ijg], writes=[B_ohj[r]])
                    P.op("dve", lambda e: e.tensor_tensor(ohi[r][:], io_i, iT[:, tg:tg + TB].unsqueeze(2).to_broadcast([128, TB, 64]), ALU.is_equal),
                         reads=[B_const, B_ijg], writes=[B_ohi[r]])
                    P.op("dve", lambda e: e.tensor_tensor(ohi[r][:], ohi[r][:], gT[:, tg:tg + TB].unsqueeze(2).to_broadcast([128, TB, 64]), ALU.mult),
                         reads=[B_ohi[r], B_ijg], writes=[B_ohi[r]])

                onehots(0)
                yield
                for tb in range(TC // TB):
                    if tb + 1 < TC // TB:
                        onehots(tb + 1)
                    r = tb % 2
                    for q8 in range(TB // 8):
                        t8 = tb * (TB // 8) + q8
                        wbk, wbb = banks[6 + t8 % 2], bbuf[6 + t8 % 2]
                        for q in range(8):
                            tok = q8 * 8 + q
                            P.op("pe", lambda e, q=q, tok=tok, wbk=wbk: e.matmul(wbk[:, q * 64:(q + 1) * 64], lhsT=ohj[r][:, tok, :], rhs=ohi[r][:, tok, :],
                                                                                 start=True, stop=True), reads=[B_ohi[r], B_ohj[r]], writes=[wbb])
                        P.op("act", lambda e, t8=t8, wbk=wbk: e.copy(Wt[buf][:, t8 * 8:(t8 + 1) * 8, :], wbk[:, :].rearrange("p (t i) -> p t i", t=8)),
                             reads=[wbb, B_Wt[buf]], writes=[B_Wt[buf]])
                    yield

            sched = [(ch, half) for ch in range(NCHK) for half in range(2)]
            for _ in wbuild_steps(0, 0, 0):
                pass
            for g_ in range(NBUF):
                load_tables(g_)
            for k, (ch, half) in enumerate(sched):
                t0 = ch * TC
                gen = wbuild_steps(sched[k + 1][0], sched[k + 1][1], (k + 1) % 2) if k + 1 < len(sched) else None
                wt_, bwt = Wt[k % 2], B_Wt[k % 2]
                if half == 1:
                    P.dma("sp", x1c[:], x1s_v[:, :, t0:t0 + TC], reads=[B_x1s], writes=[B_x1])
                pend = None
                for il in range(64 + 1):
                    i = half * 64 + il
                    if il < 64:
                        gidx = gcount["n"] + i // GI
                        b_ = gidx % NBUF
                        u_, bu = ub_[b_], B_u[b_]
                        ii = i % GI
                        abk, abb = banks[4 + i % 2], bbuf[4 + i % 2]
                        for dc in range(8):
                            P.op("pe", lambda e, dc=dc, ii=ii, abk=abk, u_=u_: e.matmul(abk[:, 0:TC], lhsT=u_[:, dc, ii * 128:(ii + 1) * 128], rhs=h2T[:, dc, t0:t0 + TC],
                                                                                        start=(dc == 0), stop=(dc == 7)), reads=[bu, B_h2], writes=[abb])
                        g_, bg = Gt[i % 2], B_G[i % 2]
                        w_, bw = WA[i % 2], B_WA[i % 2]
                        P.op("act", lambda e, abk=abk, g_=g_: e.activation(g_[:], abk[:, 0:TC], AF.Gelu), reads=[abb], writes=[bg])
                        P.op("pool", lambda e, il=il, g_=g_, w_=w_, wt_=wt_: e.tensor_tensor(w_[:], g_[:], wt_[:, :, il], ALU.mult), reads=[bg, bwt], writes=[bw])
                    if pend is not None:
                        pi_, pw_, pbw, pg = pend
                        v_, bv = vb_[pg % NBUF], B_v[pg % NBUF]
                        pil = pi_ % GI
                        for oc in range(8):
                            P.op("pe", lambda e, oc=oc, pil=pil, pi_=pi_, pw_=pw_, v_=v_: e.matmul(
                                banks[oc // 2][:, (oc % 2) * TC:(oc % 2) * TC + TC], lhsT=v_[:, pil, oc * 128:(oc + 1) * 128], rhs=pw_[:],
                                start=(pi_ == 0 and oc % 2 == 0), stop=(pi_ == 127), skip_group_check=True), reads=[bv, pbw], writes=[bbuf[oc // 2]])
                        if pi_ % GI == GI - 1:
                            nxt = pg + NBUF
                            if nxt < NCHK * NG:
                                load_tables(nxt)
                    pend = (i, w_, bw, gidx) if il < 64 else None
                    if gen is not None and il < 64 and il % 4 == 3:
                        next(gen, None)
                if gen is not None:
                    for _ in gen:
                        pass
                if half == 0:
                    continue
                gcount["n"] += NG
                for oc in range(8):
                    P.op("dve", lambda e, oc=oc: e.scalar_tensor_tensor(x1c[:, oc, :], banks[oc // 2][:, (oc % 2) * TC:(oc % 2) * TC + TC], GATE2(oc), x1c[:, oc, :],
                                                                        op0=ALU.mult, op1=ALU.add), reads=[bbuf[oc // 2], B_mod, B_x1], writes=[B_x1])
                rot["set"] = [6, 7]
                rms_rstd(lambda dc: x1c[:, dc, :], TC, rstd[:], B_x1, B_rstd)
                rot["set"] = list(range(8))
                for dc in range(8):
                    P.op("dve", lambda e, dc=dc: e.scalar_tensor_tensor(x1c[:, dc, :], x1c[:, dc, :], gfin[:, dc:dc + 1], rstd[:], op0=ALU.mult, op1=ALU.mult),
                         reads=[B_x1, B_rstd, B_const], writes=[B_x1])
                P.dma("sp", outT_v[:, :, t0:t0 + TC], x1c[:], reads=[B_x1])
            P.barrier()
        late.close()
    return nc, dbg


def make_inputs(x, c, w_ada, b_ada, norm_mix_g, w_in, pool_w, pool_scale, w_branch_pool, w_branch_attn, w_out,
                norm_ffn_g, peer_wq, peer_sub_keys, peer_u, peer_v, norm_final_g):
    f = lambda a: np.ascontiguousarray(np.asarray(a, dtype=np.float32))
    x = f(x); c = f(c)
    colT = lambda v, k: f(np.asarray(v, np.float32).reshape(k, 128).T)
    w_in0 = f(w_in[0])
    perm = []
    for base in (512, 1024):
        for h in range(8):
            o = base + 64 * h
            perm += list(range(o + 8, o + 16)) + list(range(o, o + 8)) + list(range(o + 16, o + 64))
    w_sw = f(w_in0[:, perm])
    shared = dict(
        w_ada=f(w_ada[0]), w_in=w_in0, w_sw=w_sw, pool_w=f(pool_w[0]), wbp=f(w_branch_pool[0]), wba=f(w_branch_attn[0]),
        w_out=f(w_out[0]), wq=f(peer_wq[0]),
        skT=f(np.asarray(peer_sub_keys[0], np.float32).reshape(16, 128, 128).transpose(2, 0, 1)),
        uT=f(np.asarray(peer_u[0], np.float32).T), vtab=f(peer_v[0]),
        badaT=colT(b_ada[0], 48), gmixT=colT(norm_mix_g[0], 8), gffnT=colT(norm_ffn_g[0], 8), gfinT=colT(norm_final_g, 8),
        pscT=colT(pool_scale[0], 4),
    )
    half = 8
    inv = (500000.0 ** (-np.arange(half, dtype=np.float32) / half)).astype(np.float32)
    in_maps = []
    for core in range(8):
        b, j = core // 4, core % 4
        own = list(range(8 * j, 8 * j + 8))
        others = [n for n in range(32) if n not in own]
        slots = others + own
        tok = np.concatenate([np.arange(n * 256, (n + 1) * 256) for n in slots])
        xT = f(x[b].T[:, tok])
        xh = np.zeros((D, 16), np.float32)
        if j > 0:
            xh = f(x[b, 2048 * j - 16:2048 * j].T)
        ang = tok.astype(np.float32)[None, :] * inv[:, None]
        cosv = np.cos(ang).astype(np.float32); sinv = np.sin(ang).astype(np.float32)
        cF = np.ones((64, S), np.float32); sF = np.zeros((64, S), np.float32)
        cF[0:8] = cosv; cF[8:16] = cosv; sF[0:8] = -sinv; sF[8:16] = sinv
        pastb = np.array([[0.0 if n < 8 * j else -1e30 for n in others]], np.float32)
        ic = np.zeros((4, 16), np.float32)
        for g in range(4):
            w = 2 << g
            for t in range(16):
                ic[g, t] = 1.0 / min(t + 1, w) if j == 0 else 1.0 / w
        m = dict(shared)
        m.update(xT=xT, xhT=xh, cosF=f(np.concatenate([cF, cF], 0)), sinF=f(np.concatenate([sF, sF], 0)),
                 cT=colT(c[b], 8), pastb=pastb, invcnt=f(ic.reshape(1, 64)),
                 haloflag=np.array([[0.0 if j == 0 else 1.0]], np.float32))
        in_maps.append(m)
    return in_maps


_CACHE = {}


def kernel(**inputs):
    in_maps = make_inputs(**inputs)
    if "nc" not in _CACHE:
        _CACHE["nc"] = build()[0]
    res = run_bass_kernel_spmd(_CACHE["nc"], in_maps, core_ids=list(range(8)))
    out = np.zeros((2, S, D), np.float32)
    for core in range(8):
        b, j = core // 4, core % 4
        out[b, 2048 * j:2048 * (j + 1), :] = res.results[core]["outT"].T
    return out
```
